# Optimizing a Trainium2 kernel written in Bass

```python
import math
import jax, jax.numpy as jnp
from jax import lax
import numpy as np

D_MODEL = 2048
BATCH = 1
SEQ = 8192
DEPTH = 4

N_MIXERS = 4
EPS = 1e-6
ROPE_THETA = 10000.0

CONV_WIDTH = 31
POOL_WINDOWS = (2, 4, 8, 16)
POOL_GROUPS = 4
POOL_GROUP_DIM = D_MODEL // POOL_GROUPS
DIL_PATTERNS = ((128, 1), (512, 4), (2048, 16))
N_DIL_GROUPS = 3
DIL_HEADS = 8
DIL_HEAD_DIM = 128
MLA_HEADS = 16
MLA_Q_RANK = 512
MLA_KV_RANK = 512
MLA_NOPE = 128
MLA_ROPE = 64
MLA_V = 128
MLA_QK = MLA_NOPE + MLA_ROPE
Q_BLOCK = 128
N_EXPERTS = 16
EXPERT_FF = 1024
EC_FACTOR = 2


def _n_uses(m):
    return (DEPTH - m + N_MIXERS - 1) // N_MIXERS


N_A = _n_uses(0)
N_B = _n_uses(1)
N_C = _n_uses(2)
N_D = _n_uses(3)

kernel_name = "hybrid_conv_pool_dilated_mla_ecmoe_encoder"


def rms_norm(x, g):
    xf = x.astype(jnp.float32)
    y = xf * lax.rsqrt(jnp.mean(xf * xf, axis=-1, keepdims=True) + EPS)
    return (y * g.astype(jnp.float32)).astype(x.dtype)


def rope(x, positions):
    dim = x.shape[-1]
    half = dim // 2
    inv = ROPE_THETA ** (-jnp.arange(half, dtype=jnp.float32) / half)
    ang = positions.astype(jnp.float32)[..., None] * inv
    cos = jnp.cos(ang)[:, :, None, :]
    sin = jnp.sin(ang)[:, :, None, :]
    xf = x.astype(jnp.float32)
    x1, x2 = xf[..., :half], xf[..., half:]
    return jnp.concatenate([x1 * cos - x2 * sin, x2 * cos + x1 * sin], axis=-1).astype(x.dtype)


def ada_modulation(cond, w, b):
    mod = (cond @ w + b)[:, None, :]
    shift, scale, gate = jnp.split(mod, 3, axis=-1)
    return shift, scale, gate


def conformer_conv(h, w_in, dw, ln_g, ln_b, w_out):
    a, b = jnp.split(h @ w_in, 2, axis=-1)
    u = a * jax.nn.sigmoid(b)
    u = lax.conv_general_dilated(
        u, dw[:, None, :], window_strides=(1,),
        padding=[(CONV_WIDTH // 2, CONV_WIDTH // 2)],
        dimension_numbers=("NWC", "WIO", "NWC"),
        feature_group_count=u.shape[-1])
    uf = u.astype(jnp.float32)
    mu = jnp.mean(uf, axis=-1, keepdims=True)
    var = jnp.mean(jnp.square(uf - mu), axis=-1, keepdims=True)
    un = (uf - mu) * lax.rsqrt(var + EPS) * ln_g.astype(jnp.float32) + ln_b.astype(jnp.float32)
    return jax.nn.silu(un).astype(h.dtype) @ w_out


def centred_window_mean(u, r):
    B, S, C = u.shape
    uf = u.astype(jnp.float32)
    cs = jnp.concatenate([jnp.zeros((B, 1, C), jnp.float32), jnp.cumsum(uf, axis=1)], axis=1)
    t = jnp.arange(S)
    hi = jnp.minimum(t + r + 1, S)
    lo = jnp.maximum(t - r, 0)
    return (cs[:, hi] - cs[:, lo]) / (hi - lo).astype(jnp.float32)[None, :, None]


def multiscale_pool(h, w_in, w_grp, ch_scale, w_out):
    B, S, _ = h.shape
    u = (h @ w_in).reshape(B, S, POOL_GROUPS, POOL_GROUP_DIM)
    pooled = jnp.stack(
        [centred_window_mean(u[:, :, g], w // 2) for g, w in enumerate(POOL_WINDOWS)], axis=2)
    mix = (pooled - u.astype(jnp.float32)).astype(h.dtype)
    mix = jnp.einsum("bsgc,gcd->bsgd", mix, w_grp).reshape(B, S, D_MODEL)
    return (mix * ch_scale) @ w_out


def to_phases(x, d, Lp):
    B, S = x.shape[:2]
    L = S // d
    x = jnp.moveaxis(x.reshape((B, L, d) + x.shape[2:]), 2, 1)
    return jnp.pad(x, ((0, 0), (0, 0), (0, Lp - L), (0, 0), (0, 0)))


def from_phases(t, L):
    B, d = t.shape[:2]
    t = jnp.moveaxis(t[:, :, :L], 1, 2)
    return t.reshape((B, L * d) + t.shape[3:])


def dilated_window_group(q, k, v, d, half):
    B, S, H, C = q.shape
    L = S // d
    nb = -(-L // half)
    Lp = nb * half
    qb = to_phases(q, d, Lp).reshape(B, d, nb, half, H, C)

    def band(x):
        x = jnp.pad(to_phases(x, d, Lp), ((0, 0), (0, 0), (half, half), (0, 0), (0, 0)))
        x = x.reshape(B, d, nb + 2, half, H, C)
        return jnp.concatenate([x[:, :, :-2], x[:, :, 1:-1], x[:, :, 2:]], axis=3)

    kb, vb = band(k), band(v)
    s = jnp.einsum("bpnihc,bpnjhc->bpnhij", qb, kb,
                   preferred_element_type=jnp.float32) * (C ** -0.5)
    ii = jnp.arange(half)[None, :, None]
    jj = jnp.arange(3 * half)[None, None, :]
    key_l = jnp.arange(nb)[:, None, None] * half - half + jj
    valid = (jnp.abs(ii + half - jj) <= half) & (key_l >= 0) & (key_l < L)
    s = jnp.where(valid[:, None], s, -jnp.inf)
    m = jnp.max(s, axis=-1, keepdims=True)
    p = jnp.exp(s - m)
    l = jnp.sum(p, axis=-1)
    o = jnp.einsum("bpnhij,bpnjhc->bpnihc", p, vb.astype(jnp.float32)).reshape(B, d, Lp, H, C)
    m = jnp.swapaxes(m[..., 0], 3, 4).reshape(B, d, Lp, H)
    l = jnp.swapaxes(l, 3, 4).reshape(B, d, Lp, H)
    return from_phases(o, L), from_phases(m, L), from_phases(l, L)


def dilated_attention(h, positions, w_in, q_norm, k_norm, w_out):
    B, S, _ = h.shape
    u = (h @ w_in).reshape(B, S, N_DIL_GROUPS, 3, DIL_HEADS, DIL_HEAD_DIM)
    outs, ms, ls = [], [], []
    for g, (window, dil) in enumerate(DIL_PATTERNS):
        q = rope(rms_norm(u[:, :, g, 0], q_norm[g]), positions)
        k = rope(rms_norm(u[:, :, g, 1], k_norm[g]), positions)
        o, m, l = dilated_window_group(q, k, u[:, :, g, 2], dil, window // (2 * dil))
        outs.append(o)
        ms.append(m)
        ls.append(l)
    m = jnp.stack(ms, 0)
    wgt = jnp.exp(m - jnp.max(m, axis=0, keepdims=True))
    num = jnp.einsum("gbsh,gbshc->bshc", wgt, jnp.stack(outs, 0))
    den = jnp.sum(wgt * jnp.stack(ls, 0), axis=0)
    o = (num / den[..., None]).astype(h.dtype).reshape(B, S, DIL_HEADS * DIL_HEAD_DIM)
    return o @ w_out


def mla_attention(h, positions, w_in, q_a_norm, w_q_up, kv_a_norm, w_kv_up, q_norm, k_norm, w_out):
    B, S, _ = h.shape
    u = h @ w_in
    cq = u[..., :MLA_Q_RANK]
    ckv = u[..., MLA_Q_RANK:MLA_Q_RANK + MLA_KV_RANK]
    kr = u[..., MLA_Q_RANK + MLA_KV_RANK:]
    q = (rms_norm(cq, q_a_norm) @ w_q_up).reshape(B, S, MLA_HEADS, MLA_QK)
    kv = (rms_norm(ckv, kv_a_norm) @ w_kv_up).reshape(B, S, MLA_HEADS, MLA_NOPE + MLA_V)
    k_nope, v = kv[..., :MLA_NOPE], kv[..., MLA_NOPE:]
    k = jnp.concatenate(
        [k_nope, jnp.broadcast_to(kr[:, :, None, :], (B, S, MLA_HEADS, MLA_ROPE))], axis=-1)
    q = rms_norm(q, q_norm)
    k = rms_norm(k, k_norm)
    q = jnp.concatenate([q[..., :MLA_NOPE], rope(q[..., MLA_NOPE:], positions)], axis=-1)
    k = jnp.concatenate([k[..., :MLA_NOPE], rope(k[..., MLA_NOPE:], positions)], axis=-1)
    nq = S // Q_BLOCK
    qb = jnp.moveaxis(q.reshape(B, nq, Q_BLOCK, MLA_HEADS, MLA_QK), 1, 0)
    scale = MLA_QK ** -0.5

    def attend(qblk):
        s = jnp.einsum("bqhc,bkhc->bhqk", qblk, k, preferred_element_type=jnp.float32) * scale
        p = jax.nn.softmax(s, axis=-1)
        return jnp.einsum("bhqk,bkhc->bqhc", p.astype(v.dtype), v)

    o = jnp.moveaxis(lax.map(attend, qb), 0, 1).reshape(B, S, MLA_HEADS * MLA_V)
    return o @ w_out


def expert_choice_ffn(h, w_router, w_gate, w_up, w_down):
    B, S, _ = h.shape
    cap = EC_FACTOR * S // N_EXPERTS
    logits = jnp.einsum("bsd,de->bse", h, w_router, preferred_element_type=jnp.float32)
    aff = jax.nn.softmax(logits, axis=-1)
    gates, idx = lax.top_k(jnp.swapaxes(aff, 1, 2), cap)
    bidx = jnp.arange(B)[:, None, None]
    xs = h[bidx, idx]
    a = jnp.einsum("becd,edf->becf", xs, w_gate)
    b = jnp.einsum("becd,edf->becf", xs, w_up)
    y = jnp.einsum("becf,efd->becd", jax.nn.silu(a) * b, w_down)
    y = y * gates[..., None].astype(y.dtype)
    return jnp.zeros_like(h).at[bidx, idx].add(y)


def setup_inputs(seed: int = 0) -> dict:
    key = jax.random.key(seed)
    ks = iter(jax.random.split(key, 40))

    def nrm(shape, scale):
        return jax.random.normal(next(ks), shape, jnp.float32) * scale

    def gain(shape):
        return 1.0 + nrm(shape, 0.1)

    D = D_MODEL
    dil_cols = N_DIL_GROUPS * 3 * DIL_HEADS * DIL_HEAD_DIM
    mla_cols = MLA_Q_RANK + MLA_KV_RANK + MLA_ROPE
    return {
        "x": nrm((BATCH, SEQ, D), 1.0),
        "c": nrm((BATCH, D), 1.0),
        "positions": jnp.broadcast_to(jnp.arange(SEQ, dtype=jnp.int32), (BATCH, SEQ)),
        "norm_g": gain((DEPTH, 2, D)),
        "ada_w": nrm((DEPTH, 2, D, 3 * D), 0.5 * D ** -0.5),
        "ada_b": nrm((DEPTH, 2, 3 * D), 0.02),
        "conv_w_in": nrm((N_A, D, 2 * D), D ** -0.5),
        "conv_dw": nrm((N_A, CONV_WIDTH, D), CONV_WIDTH ** -0.5),
        "conv_ln_g": gain((N_A, D)),
        "conv_ln_b": nrm((N_A, D), 0.02),
        "conv_w_out": nrm((N_A, D, D), D ** -0.5),
        "pool_w_in": nrm((N_B, D, D), D ** -0.5),
        "pool_w_grp": nrm((N_B, POOL_GROUPS, POOL_GROUP_DIM, POOL_GROUP_DIM), POOL_GROUP_DIM ** -0.5),
        "pool_scale": gain((N_B, D)),
        "pool_w_out": nrm((N_B, D, D), D ** -0.5),
        "dil_w_in": nrm((N_C, D, dil_cols), D ** -0.5),
        "dil_q_norm": gain((N_C, N_DIL_GROUPS, DIL_HEAD_DIM)),
        "dil_k_norm": gain((N_C, N_DIL_GROUPS, DIL_HEAD_DIM)),
        "dil_w_out": nrm((N_C, DIL_HEADS * DIL_HEAD_DIM, D), (DIL_HEADS * DIL_HEAD_DIM) ** -0.5),
        "mla_w_in": nrm((N_D, D, mla_cols), D ** -0.5),
        "mla_q_a_norm": gain((N_D, MLA_Q_RANK)),
        "mla_w_q_up": nrm((N_D, MLA_Q_RANK, MLA_HEADS * MLA_QK), MLA_Q_RANK ** -0.5),
        "mla_kv_a_norm": gain((N_D, MLA_KV_RANK)),
        "mla_w_kv_up": nrm((N_D, MLA_KV_RANK, MLA_HEADS * (MLA_NOPE + MLA_V)), MLA_KV_RANK ** -0.5),
        "mla_q_norm": gain((N_D, MLA_QK)),
        "mla_k_norm": gain((N_D, MLA_QK)),
        "mla_w_out": nrm((N_D, MLA_HEADS * MLA_V, D), (MLA_HEADS * MLA_V) ** -0.5),
        "moe_router": nrm((DEPTH, D, N_EXPERTS), D ** -0.5),
        "moe_w_gate": nrm((DEPTH, N_EXPERTS, D, EXPERT_FF), D ** -0.5),
        "moe_w_up": nrm((DEPTH, N_EXPERTS, D, EXPERT_FF), D ** -0.5),
        "moe_w_down": nrm((DEPTH, N_EXPERTS, EXPERT_FF, D), EXPERT_FF ** -0.5),
    }


def reference(x, c, positions, norm_g, ada_w, ada_b,
              conv_w_in, conv_dw, conv_ln_g, conv_ln_b, conv_w_out,
              pool_w_in, pool_w_grp, pool_scale, pool_w_out,
              dil_w_in, dil_q_norm, dil_k_norm, dil_w_out,
              mla_w_in, mla_q_a_norm, mla_w_q_up, mla_kv_a_norm, mla_w_kv_up,
              mla_q_norm, mla_k_norm, mla_w_out,
              moe_router, moe_w_gate, moe_w_up, moe_w_down):
    cond = jax.nn.silu(c)
    for layer in range(DEPTH):
        mixer = layer % N_MIXERS
        occ = layer // N_MIXERS
        shift, scale, gate = ada_modulation(cond, ada_w[layer, 0], ada_b[layer, 0])
        h = rms_norm(x, norm_g[layer, 0]) * (1.0 + scale) + shift
        if mixer == 0:
            y = conformer_conv(h, conv_w_in[occ], conv_dw[occ], conv_ln_g[occ], conv_ln_b[occ],
                               conv_w_out[occ])
        elif mixer == 1:
            y = multiscale_pool(h, pool_w_in[occ], pool_w_grp[occ], pool_scale[occ], pool_w_out[occ])
        elif mixer == 2:
            y = dilated_attention(h, positions, dil_w_in[occ], dil_q_norm[occ], dil_k_norm[occ],
                                  dil_w_out[occ])
        else:
            y = mla_attention(h, positions, mla_w_in[occ], mla_q_a_norm[occ], mla_w_q_up[occ],
                              mla_kv_a_norm[occ], mla_w_kv_up[occ], mla_q_norm[occ],
                              mla_k_norm[occ], mla_w_out[occ])
        x = x + (1.0 + gate) * y
        shift, scale, gate = ada_modulation(cond, ada_w[layer, 1], ada_b[layer, 1])
        h = rms_norm(x, norm_g[layer, 1]) * (1.0 + scale) + shift
        y = expert_choice_ffn(h, moe_router[layer], moe_w_gate[layer], moe_w_up[layer],
                              moe_w_down[layer])
        x = x + (1.0 + gate) * y
    return x
```

```python
import numpy as np
import ml_dtypes
from contextlib import ExitStack
import concourse.bass as bass
import concourse.mybir as mybir
from concourse.bass_utils import run_bass_kernel_spmd

F32 = mybir.dt.float32
BF16 = mybir.dt.bfloat16
I32 = mybir.dt.int32
AF = mybir.ActivationFunctionType
ALU = mybir.AluOpType
AX = mybir.AxisListType
NPBF = ml_dtypes.bfloat16

NCORES = 8
D = 2048
S = 8192
T = 1024
KC = 16
EPS = 1e-6


class TK:
    NDS = 24

    def __init__(self, nc, es):
        self.nc = nc
        self.es = es
        self.engs = {"pe": nc.tensor, "act": nc.scalar, "dve": nc.vector,
                     "pool": nc.gpsimd, "sp": nc.sync}
        self.esem = {k: es.enter_context(nc.semaphore("sem_" + k)) for k in ("pe", "act", "dve", "pool")}
        self.ecnt = {k: 0 for k in self.esem}
        self.dsem = [es.enter_context(nc.semaphore("dsem%d" % i)) for i in range(self.NDS)]
        self.dcnt = [0] * self.NDS
        self.di = 0
        self.seen = {k: {} for k in self.engs}
        self.lastw = {}
        self.accw = {}
        self.reads = {}
        self.out_evs = []
        self.nsb = 0

    def sb(self, shape, dtype, name=None):
        self.nsb += 1
        return self.es.enter_context(self.nc.sbuf_tensor(name or ("sb%d" % self.nsb), list(shape), dtype))

    def ps(self, shape, dtype, name=None):
        self.nsb += 1
        return self.es.enter_context(self.nc.psum_tensor(name or ("ps%d" % self.nsb), list(shape), dtype))

    def _wait(self, eng, evs):
        seen = self.seen[eng]
        best = {}
        for (sem, val) in evs:
            if val <= 0:
                continue
            key = id(sem)
            if seen.get(key, 0) >= val:
                continue
            if key not in best or best[key][1] < val:
                best[key] = (sem, val)
        for key, (sem, val) in best.items():
            self.engs[eng].wait_ge(sem, val)
            seen[key] = val

    def _deps(self, eng, r, w, wacc=()):
        deps = []
        for k in r:
            if k in self.lastw:
                deps.append(self.lastw[k])
            deps.extend(self.accw.get(k, {}).values())
        for k in w:
            if k in self.lastw:
                deps.append(self.lastw[k])
            deps.extend(self.accw.get(k, {}).values())
            deps.extend(self.reads.get(k, {}).values())
        for k in wacc:
            if k in self.lastw:
                deps.append(self.lastw[k])
            deps.extend(self.reads.get(k, {}).values())
        if eng == "pe":
            own = id(self.esem["pe"])
            deps = [d for d in deps if id(d[0]) != own]
        return deps

    def _record(self, ev, r, w, wacc=()):
        for k in wacc:
            d = self.accw.setdefault(k, {})
            key = id(ev[0])
            if key not in d or d[key][1] < ev[1]:
                d[key] = ev
        for k in r:
            d = self.reads.setdefault(k, {})
            key = id(ev[0])
            if key not in d or d[key][1] < ev[1]:
                d[key] = ev
        for k in w:
            self.lastw[k] = ev
            self.reads[k] = {}
            self.accw[k] = {}

    def op(self, eng, fn, r=(), w=()):
        self._wait(eng, self._deps(eng, r, w))
        ins = fn(self.engs[eng])
        self.ecnt[eng] += 1
        ins.then_inc(self.esem[eng], 1)
        ev = (self.esem[eng], self.ecnt[eng])
        self._record(ev, r, w)
        return ev

    def dma(self, q, out, in_, r=(), w=(), is_output=False, **kw):
        self._wait(q, self._deps(q, r, w))
        i = self.di % self.NDS
        self.di += 1
        if self.dcnt[i] > 0:
            self._wait(q, [(self.dsem[i], self.dcnt[i])])
        ins = self.engs[q].dma_start(out=out, in_=in_, **kw)
        self.dcnt[i] += 16
        ins.then_inc(self.dsem[i], 16)
        ev = (self.dsem[i], self.dcnt[i])
        self._record(ev, r, w)
        if is_output:
            self.out_evs.append(ev)
        return ev

    def idma(self, out, out_off, in_, in_off, r=(), w=(), wacc=(), is_output=False, **kw):
        q = "pool"
        self._wait(q, self._deps(q, r, w, wacc))
        i = self.di % self.NDS
        self.di += 1
        if self.dcnt[i] > 0:
            self._wait(q, [(self.dsem[i], self.dcnt[i])])
        ins = self.nc.gpsimd.indirect_dma_start(out=out, out_offset=out_off, in_=in_, in_offset=in_off, **kw)
        self.dcnt[i] += 16
        ins.then_inc(self.dsem[i], 16)
        ev = (self.dsem[i], self.dcnt[i])
        self._record(ev, r, w, wacc)
        if is_output:
            self.out_evs.append(ev)
        return ev

    def finish(self):
        evs = list(self.out_evs)
        for i in range(self.NDS):
            if self.dcnt[i] > 0:
                evs.append((self.dsem[i], self.dcnt[i]))
        for k in self.esem:
            if self.ecnt[k] > 0:
                evs.append((self.esem[k], self.ecnt[k]))
        self._wait("sp", evs)


def new_nc():
    return bass.Bass("TRN2", target_bir_lowering=False)


def din(nc, name, shape, dtype):
    return nc.dram_tensor(name, list(shape), dtype, kind="ExternalInput").ap()


def dout(nc, name, shape, dtype):
    return nc.dram_tensor(name, list(shape), dtype, kind="ExternalOutput").ap()


def run(nc, in_maps):
    res = run_bass_kernel_spmd(nc, in_maps, core_ids=list(range(NCORES)))
    return res.results


def build_ada():
    nc = new_nc()
    cT = din(nc, "cT", [128, KC], F32)
    w = din(nc, "w", [D, 3 * D], F32)
    b = din(nc, "b", [1, 3 * D], F32)
    o = dout(nc, "mod", [1, 3 * D], F32)
    es = ExitStack()
    with es:
        tk = TK(nc, es)
        cs = tk.sb([128, KC], F32)
        cond = tk.sb([128, KC], F32)
        bsb = tk.sb([1, 3 * D], F32)
        osb = tk.sb([1, 3 * D], F32)
        wb = [tk.sb([128, KC, 512], F32) for _ in range(2)]
        pss = [tk.ps([128, 512], F32) for _ in range(2)]
        tk.dma("sp", cs[:], cT, w=["cs"])
        tk.dma("sp", bsb[:], b, w=["b"])
        tk.op("act", lambda e: e.activation(out=cond[:], in_=cs[:], func=AF.Silu), r=["cs"], w=["cond"])
        wv = w.rearrange("(kc p) f -> p kc f", p=128)
        for n in range(12):
            wbuf = wb[n % 2]
            tk.dma("sp", wbuf[:], wv[:, :, n * 512:(n + 1) * 512], w=["wb%d" % (n % 2)])
            ps = pss[n % 2]
            for kc in range(KC):
                tk.op("pe", lambda e, kc=kc: e.matmul(ps[0:1, :], cond[:, kc:kc + 1], wbuf[:, kc, :],
                                                      start=(kc == 0), stop=(kc == KC - 1)),
                      r=["cond", "wb%d" % (n % 2)], w=["ps%d" % (n % 2)])
            tk.op("dve", lambda e: e.tensor_tensor(out=osb[0:1, n * 512:(n + 1) * 512], in0=ps[0:1, :],
                                                   in1=bsb[0:1, n * 512:(n + 1) * 512], op=ALU.add),
                  r=["ps%d" % (n % 2), "b"], w=["osb%d" % n])
        tk.dma("sp", o, osb[:], r=["osb%d" % n for n in range(12)], is_output=True)
        tk.finish()
    return nc


def fm16(v):
    return np.ascontiguousarray(np.asarray(v).reshape(KC, 128).T)


def run_ada(c, ada_w, ada_b):
    nc = build_ada()
    cT = fm16(c[0])
    maps = []
    for r in range(NCORES):
        l, s = divmod(r, 2)
        maps.append({"cT": cT, "w": np.ascontiguousarray(ada_w[l, s]), "b": np.ascontiguousarray(ada_b[l, s][None, :])})
    res = run(nc, maps)
    return np.stack([res[r]["mod"][0] for r in range(NCORES)]).reshape(4, 2, 3 * D)


class Ctx:
    def __init__(self, nc, es, n_wbuf=3, wcols=512, wkc=KC, n_ps=6, n_psb=2):
        self.nc = nc
        self.tk = TK(nc, es)
        tk = self.tk
        self.psum = [tk.ps([128, 512], F32, name="psum%d" % i) for i in range(n_ps)]
        self.psi = 0
        self.psb = [tk.ps([128, 1024], BF16, name="psumb%d" % i) for i in range(n_psb)]
        self.psbi = 0
        self.wb = [tk.sb([128, wkc, wcols], BF16, name="wblk%d" % i) for i in range(n_wbuf)]
        self.wbi = 0
        self.ones = tk.sb([128, 128], BF16, name="ones_bf")
        tk.op("dve", lambda e: e.memset(self.ones[:], 1.0), w=["ones"])
        self.uid = 0
        self.epsD = tk.sb([128, 1], F32, name="epsD")
        tk.op("dve", lambda e: e.memset(self.epsD[:], float(D * EPS)), w=["epsD"])
        self.rm_sq = [tk.sb([128, 512], BF16) for _ in range(2)]
        self.rm_tmp = [tk.sb([128, 512], F32) for _ in range(2)]
        self.rm_hf = [tk.sb([128, 512], F32) for _ in range(2)]
        self.rm_rstd = tk.sb([128, 512], F32)

    def next_ps(self):
        i = self.psi % len(self.psum)
        self.psi += 1
        return self.psum[i], "psum%d" % i

    def next_psb(self):
        i = self.psbi % len(self.psb)
        self.psbi += 1
        return self.psb[i], "psumb%d" % i

    def next_wb(self):
        i = self.wbi % len(self.wb)
        self.wbi += 1
        return self.wb[i], "wblk%d" % i

    def key(self, s):
        self.uid += 1
        return "%s_%d" % (s, self.uid)


def tchunks(TT, n=512):
    return [(a, min(n, TT - a)) for a in range(0, TT, n)]


def linear_fm(cx, W, in_tile, in_keys, nk, out_chunks, TT, evac, t0=0):
    tk = cx.tk
    Wv = W.rearrange("(kc p) f -> p kc f", p=128)
    wcols = cx.wb[0].shape[2]
    blocks = []
    for ci, (c0, M) in enumerate(out_chunks):
        if blocks and blocks[-1][0] + blocks[-1][1] == c0 and blocks[-1][1] + M <= wcols:
            blocks[-1][1] += M
            blocks[-1][2].append((ci, c0, M))
        else:
            blocks.append([c0, M, [(ci, c0, M)]])
    for (b0, bw, chunks) in blocks:
        wb, wkey = cx.next_wb()
        tk.dma("pool", wb[:, 0:nk, 0:bw], Wv[:, :, b0:b0 + bw], w=[wkey])
        for (ci, c0, M) in chunks:
            for (tc0, n) in tchunks(TT):
                ps, pkey = cx.next_ps()
                for kc in range(nk):
                    tk.op("pe", lambda e, kc=kc: e.matmul(ps[0:M, 0:n], wb[:, kc, c0 - b0:c0 - b0 + M],
                                                          in_tile[:, kc, t0 + tc0:t0 + tc0 + n],
                                                          start=(kc == 0), stop=(kc == nk - 1)),
                          r=[wkey, in_keys(kc)], w=[pkey])
                evac(ci, c0, M, tc0, n, ps, pkey)


def load_mod(cx, mod_ap, g_ap):
    tk = cx.tk
    k = cx.key("mod")
    m = tk.sb([128, 48], F32)
    g = tk.sb([128, 16], F32)
    gs = tk.sb([128, 16], F32)
    g1 = tk.sb([128, 16], F32)
    tk.dma("sp", m[:], mod_ap, w=[k + "m"])
    tk.dma("sp", g[:], g_ap, w=[k + "g"])
    tk.op("dve", lambda e: e.scalar_tensor_tensor(out=gs[:], in0=m[:, 16:32], scalar=1.0, in1=g[:],
                                                  op0=ALU.add, op1=ALU.mult), r=[k + "m", k + "g"], w=[k + "gs0"])
    tk.op("dve", lambda e: e.tensor_scalar(out=gs[:], in0=gs[:], scalar1=float(np.sqrt(D)), scalar2=None,
                                           op0=ALU.mult), r=[k + "gs0"], w=[k + "gs"])
    tk.op("dve", lambda e: e.tensor_scalar(out=g1[:], in0=m[:, 32:48], scalar1=1.0, scalar2=None,
                                           op0=ALU.add), r=[k + "m"], w=[k + "g1"])
    return {"shift": m[:, 0:16], "gs": gs, "g1": g1, "kshift": k + "m", "kgs": k + "gs", "kg1": k + "g1"}


def rms_mod(cx, xs, xkeys, TT, t0, mod, h_out, hkey, router=None):
    tk = cx.tk
    sq, tmp, hf, rstd = cx.rm_sq, cx.rm_tmp, cx.rm_hf, cx.rm_rstd
    base = "rm"
    for ti, (tc0, n) in enumerate(tchunks(TT)):
        ps, pkey = cx.next_ps()
        for kc in range(KC):
            s = sq[kc % 2]
            sk = base + "sq%d" % (kc % 2)
            tk.op("act", lambda e: e.activation(out=s[:, 0:n], in_=xs[:, kc, t0 + tc0:t0 + tc0 + n], func=AF.Square),
                  r=[xkeys(kc)], w=[sk])
            tk.op("pe", lambda e: e.matmul(ps[:, 0:n], cx.ones[:], s[:, 0:n], start=(kc == 0), stop=(kc == KC - 1)),
                  r=[sk, "ones"], w=[pkey])
        rk = base + "rstd"
        tk.op("act", lambda e: e.activation(out=rstd[:, 0:n], in_=ps[:, 0:n], func=AF.Sqrt, bias=cx.epsD[:, 0:1], scale=1.0),
              r=[pkey, "epsD"], w=[rk + "0"])
        tk.op("dve", lambda e: e.reciprocal(out=rstd[:, 0:n], in_=rstd[:, 0:n]), r=[rk + "0"], w=[rk])
        for kc in range(KC):
            tm = tmp[kc % 2]
            tkk = base + "tmp%d" % (kc % 2)
            tk.op("dve", lambda e: e.tensor_tensor(out=tm[:, 0:n], in0=xs[:, kc, t0 + tc0:t0 + tc0 + n], in1=rstd[:, 0:n],
                                                   op=ALU.mult), r=[xkeys(kc), rk], w=[tkk])
            tk.op("act", lambda e: e.activation(out=h_out[:, kc, tc0:tc0 + n], in_=tm[:, 0:n], func=AF.Identity,
                                                bias=mod["shift"][:, kc:kc + 1], scale=mod["gs"][:, kc:kc + 1]),
                  r=[tkk, mod["kshift"], mod["kgs"]], w=[hkey(kc)])
            if router:
                hh = hf[kc % 2]
                hk = base + "hf%d" % (kc % 2)
                tk.op("act", lambda e: e.activation(out=hh[:, 0:n], in_=tm[:, 0:n], func=AF.Identity,
                                                    bias=mod["shift"][:, kc:kc + 1], scale=mod["gs"][:, kc:kc + 1]),
                      r=[tkk, mod["kshift"], mod["kgs"]], w=[hk])
                rps, rpk = router["ps"][ti], router["pkeys"][ti]
                tk.op("pe", lambda e: e.matmul(rps[0:16, 0:n], router["w"][:, kc, :], hh[:, 0:n],
                                               start=(kc == 0), stop=(kc == KC - 1)),
                      r=[hk, router["key"]], w=[rpk])


def load_xT(cx, xT_ap, TT):
    tk = cx.tk
    xs = tk.sb([128, KC, TT], F32, name="xs")
    xv = xT_ap.rearrange("(kc p) t -> p kc t", p=128)
    for kc in range(KC):
        tk.dma("sp", xs[:, kc, :], xv[:, kc, :], w=["x%d" % kc])
    return xs


def epilogue(cx, xs, halo, y_in, y_keys, nk_out, w_out_ap, modA, modB_ap, gB_ap, router_ap, outs, h2, h2key):
    tk = cx.tk

    def evac(ci, c0, M, tc0, n, ps, pkey):
        f = c0 // 128
        tk.op("dve", lambda e: e.scalar_tensor_tensor(out=xs[:, f, halo + tc0:halo + tc0 + n], in0=ps[:, 0:n],
                                                      scalar=modA["g1"][:, f:f + 1], in1=xs[:, f, halo + tc0:halo + tc0 + n],
                                                      op0=ALU.mult, op1=ALU.add),
              r=[pkey, modA["kg1"]], w=["x%d" % f])

    linear_fm(cx, w_out_ap, y_in, y_keys, nk_out, [(f * 128, 128) for f in range(KC)], T, evac)
    x1v = outs["x1T"].rearrange("(kc p) t -> p kc t", p=128)
    for kc in range(KC):
        tk.dma("sp", x1v[:, kc, :], xs[:, kc, halo:halo + T], r=["x%d" % kc], is_output=True)
    modB = load_mod(cx, modB_ap, gB_ap)
    rw = tk.sb([128, KC, 16], F32, name="router_w")
    tk.dma("sp", rw[:], router_ap.rearrange("(kc p) e -> p kc e", p=128), w=["router_w"])
    rps = []
    rpk = []
    for _ in tchunks(T):
        p, k = cx.next_ps()
        rps.append(p)
        rpk.append(k)
    rms_mod(cx, xs, lambda kc: "x%d" % kc, T, halo, modB, h2, h2key,
            router={"w": rw, "key": "router_w", "ps": rps, "pkeys": rpk})
    h2v = outs["h2T"].rearrange("(kc p) t -> p kc t", p=128)
    for kc in range(KC):
        tk.dma("sp", h2v[:, kc, :], h2[:, kc, 0:T], r=[h2key(kc)], is_output=True)
    lg = tk.sb([16, T], F32, name="lg")
    for ti, (tc0, n) in enumerate(tchunks(T)):
        tk.op("dve", lambda e: e.tensor_copy(lg[0:16, tc0:tc0 + n], rps[ti][0:16, 0:n]), r=[rpk[ti]], w=["lg%d" % ti])
    tk.dma("sp", outs["lgT"], lg[:], r=["lg%d" % ti for ti in range(len(tchunks(T)))], is_output=True)


def m_common_io(nc, halo):
    TT = T + 2 * halo
    io = {
        "xT": din(nc, "xT", [D, TT], F32),
        "modA": din(nc, "modA", [128, 48], F32),
        "modB": din(nc, "modB", [128, 48], F32),
        "gA": din(nc, "gA", [128, 16], F32),
        "gB": din(nc, "gB", [128, 16], F32),
        "router": din(nc, "router", [D, 16], F32),
        "x1T": dout(nc, "x1T", [D, T], F32),
        "h2T": dout(nc, "h2T", [D, T], BF16),
        "lgT": dout(nc, "lgT", [16, T], F32),
    }
    return io


POOL_HALO = 8


def build_m_pool():
    nc = new_nc()
    halo = POOL_HALO
    TT = T + 2 * halo
    io = m_common_io(nc, halo)
    w_in = din(nc, "w_in", [D, D], F32)
    w_grp = din(nc, "w_grp", [4, 512, 512], F32)
    ch_scale = din(nc, "ch_scale", [128, 16], F32)
    w_out = din(nc, "w_out", [D, D], F32)
    vmask = din(nc, "vmask", [1, TT], F32)
    invcnt = din(nc, "invcnt", [1, 4 * 16], F32)
    es = ExitStack()
    with es:
        cx = Ctx(nc, es, n_wbuf=2)
        tk = cx.tk
        xs = load_xT(cx, io["xT"], TT)
        modA = load_mod(cx, io["modA"], io["gA"])
        csc = tk.sb([128, 16], F32)
        tk.dma("sp", csc[:], ch_scale, w=["csc"])
        vm = tk.sb([128, TT], F32)
        tk.dma("sp", vm[:], vmask.to_broadcast([128, TT]), w=["vm"])
        ic = tk.sb([128, 64], F32)
        tk.dma("sp", ic[:], invcnt.to_broadcast([128, 64]), w=["ic"])
        bufA = tk.sb([128, KC, TT], BF16, name="bufA")
        bufB = tk.sb([128, KC, T], BF16, name="bufB")
        kA = lambda kc: "A%d" % kc
        kB = lambda kc: "B%d" % kc
        hT = bufA
        rms_mod(cx, xs, lambda kc: "x%d" % kc, TT, 0, modA, hT, kA)
        mixT = bufB
        ub = [tk.sb([128, TT], F32) for _ in range(2)]
        s1 = tk.sb([128, TT], F32)
        s2 = tk.sb([128, TT], F32)
        e8 = tk.sb([128, 16], F32)

        def evac_u(ci, c0, M, tc0, n, ps, pkey):
            f = ci
            u = ub[f % 2]
            uk = "u%d" % (f % 2)
            tk.op("act", lambda e: e.activation(out=u[:, tc0:tc0 + n], in_=ps[:, 0:n], func=AF.Copy), r=[pkey], w=[uk + "_%d" % tc0])
            if tc0 + n == TT:
                ukeys = [uk + "_%d" % a for (a, _) in tchunks(TT)]
                g = f // 4
                r = (1, 2, 4, 8)[g]
                tk.op("dve", lambda e: e.tensor_tensor(out=u[:, 0:halo], in0=u[:, 0:halo], in1=vm[:, 0:halo], op=ALU.mult),
                      r=ukeys + ["vm"], w=[uk + "_0"])
                tk.op("dve", lambda e: e.tensor_tensor(out=u[:, TT - halo:TT], in0=u[:, TT - halo:TT], in1=vm[:, TT - halo:TT], op=ALU.mult),
                      r=ukeys + ["vm"], w=[ukeys[-1]])
                src, srck, wdt = u, ukeys, 1
                bufs = [(s1, "s1"), (s2, "s2")]
                bi = 0
                while wdt < 2 * r:
                    dst, dk = bufs[bi % 2]
                    bi += 1
                    L = TT - 2 * wdt + 1
                    tk.op("dve", lambda e, src=src, dst=dst, wdt=wdt, L=L: e.tensor_tensor(
                        out=dst[:, 0:L], in0=src[:, 0:L], in1=src[:, wdt:wdt + L], op=ALU.add), r=srck, w=[dk])
                    src, srck, wdt = dst, [dk], 2 * wdt
                dst, dk = bufs[bi % 2]
                tk.op("dve", lambda e: e.tensor_tensor(out=dst[:, 0:T], in0=src[:, halo - r:halo - r + T],
                                                       in1=u[:, halo + r:halo + r + T], op=ALU.add), r=srck + ukeys, w=[dk])
                tk.op("dve", lambda e: e.scalar_tensor_tensor(out=mixT[:, f, :], in0=dst[:, 0:T], scalar=1.0 / (2 * r + 1),
                                                              in1=u[:, halo:halo + T], op0=ALU.mult, op1=ALU.subtract),
                      r=[dk] + ukeys, w=[kB(f)])
                for (a, ia) in ((0, 0), (T - 8, 8)):
                    tk.op("dve", lambda e, a=a, ia=ia: e.tensor_tensor(out=e8[:, ia:ia + 8], in0=dst[:, a:a + 8],
                                                                       in1=ic[:, g * 16 + ia:g * 16 + ia + 8], op=ALU.mult),
                          r=[dk, "ic"], w=["e8_%d" % ia])
                    tk.op("dve", lambda e, a=a, ia=ia: e.tensor_tensor(out=mixT[:, f, a:a + 8], in0=e8[:, ia:ia + 8],
                                                                       in1=u[:, halo + a:halo + a + 8], op=ALU.subtract),
                          r=["e8_%d" % ia] + ukeys, w=[kB(f)])

        linear_fm(cx, w_in, hT, kA, KC, [(f * 128, 128) for f in range(KC)], TT, evac_u)
        m2T = bufA
        for g in range(4):
            def evac_g(ci, c0, M, tc0, n, ps, pkey, g=g):
                f = 4 * g + ci
                tk.op("act", lambda e: e.activation(out=m2T[:, f, tc0:tc0 + n], in_=ps[:, 0:n], func=AF.Copy,
                                                    scale=csc[:, f:f + 1]), r=[pkey, "csc"], w=[kA(f)])
            linear_fm(cx, w_grp[g], mixT[:, 4 * g:4 * g + 4, :], lambda kc, g=g: kB(4 * g + kc), 4,
                      [(j * 128, 128) for j in range(4)], T, evac_g)
        epilogue(cx, xs, halo, m2T, kA, KC, w_out, modA, io["modB"], io["gB"], io["router"], io, bufB, kB)
        tk.finish()
    return nc


def mod48(m):
    return np.ascontiguousarray(np.concatenate([fm16(m[0:D]), fm16(m[D:2 * D]), fm16(m[2 * D:3 * D])], axis=1))


def shard_T(xT_full, r, halo):
    lo, hi = r * T - halo, (r + 1) * T + halo
    out = np.zeros((xT_full.shape[0], T + 2 * halo), xT_full.dtype)
    a, b = max(lo, 0), min(hi, S)
    out[:, a - lo:b - lo] = xT_full[:, a:b]
    return out


def common_maps(xT_full, inp, mod, layer, halo):
    maps = []
    for r in range(NCORES):
        maps.append({
            "xT": shard_T(xT_full, r, halo),
            "modA": mod48(mod[layer, 0]), "modB": mod48(mod[layer, 1]),
            "gA": fm16(inp["norm_g"][layer, 0]), "gB": fm16(inp["norm_g"][layer, 1]),
            "router": np.ascontiguousarray(inp["moe_router"][layer]),
        })
    return maps


def gather_common(res):
    x1T = np.concatenate([res[r]["x1T"] for r in range(NCORES)], axis=1)
    h2T = np.concatenate([res[r]["h2T"] for r in range(NCORES)], axis=1)
    lgT = np.concatenate([res[r]["lgT"] for r in range(NCORES)], axis=1)
    return x1T, h2T, lgT


_NC_CACHE = {}


def get_nc(name, builder):
    if name not in _NC_CACHE:
        _NC_CACHE[name] = builder()
    return _NC_CACHE[name]


def run_m_pool(xT_full, inp, mod, layer):
    nc = get_nc("m_pool", build_m_pool)
    halo = POOL_HALO
    maps = common_maps(xT_full, inp, mod, layer, halo)
    pos = np.arange(S)
    for r in range(NCORES):
        lo = r * T - halo
        tt = np.arange(lo, lo + T + 2 * halo)
        vmask = ((tt >= 0) & (tt < S)).astype(np.float32)[None, :]
        ic = np.zeros((4, 16), np.float32)
        for g, rr in enumerate((1, 2, 4, 8)):
            for ia, a in ((0, 0), (8, T - 8)):
                t = r * T + a + np.arange(8)
                cnt = np.minimum(t + rr + 1, S) - np.maximum(t - rr, 0)
                ic[g, ia:ia + 8] = 1.0 / cnt.astype(np.float32)
        maps[r].update({
            "w_in": np.ascontiguousarray(inp["pool_w_in"][0]), "w_grp": np.ascontiguousarray(inp["pool_w_grp"][0]),
            "ch_scale": fm16(inp["pool_scale"][0]), "w_out": np.ascontiguousarray(inp["pool_w_out"][0]),
            "vmask": vmask, "invcnt": ic.reshape(1, 64),
        })
    return gather_common(run(nc, maps))


CAP = 1024
BIGSLOT = 16384.0
BIS_ITERS = 30


def build_moe():
    nc = new_nc()
    lg = din(nc, "lg", [S, 16], F32)
    h2 = din(nc, "h2", [S, D], BF16)
    wg = din(nc, "wg", [2, D, 1024], F32)
    wu = din(nc, "wu", [2, D, 1024], F32)
    wd = din(nc, "wd", [2, 1024, D], F32)
    ltri = din(nc, "ltri", [128, 128], F32)
    ident = din(nc, "ident", [128, 128], F32)
    Y = dout(nc, "Y", [2, CAP, D], BF16)
    inv = dout(nc, "inv", [2, S], I32)
    idxl = [nc.dram_tensor("idxl%d" % i, [CAP + 8, 2], I32).ap() for i in range(2)]
    es = ExitStack()
    with es:
        cx = Ctx(nc, es, n_wbuf=4)
        tk = cx.tk
        lgs = tk.sb([128, 64, 16], F32)
        tk.dma("sp", lgs[:], lg.rearrange("(p j) e -> p j e", j=64), w=["lgs"])
        lt = tk.sb([128, 128], F32)
        tk.dma("sp", lt[:], ltri, w=["lt"])
        idb = tk.sb([128, 128], BF16)
        tk.dma("pool", idb[:], ident, w=["idb"])
        wdsb = tk.sb([128, 8, D], BF16, name="wdsb")

        def load_gu(e_):
            wgv = wg[e_].rearrange("(kc p) f -> p kc f", p=128)
            wuv = wu[e_].rearrange("(kc p) f -> p kc f", p=128)
            out = []
            for fb in range(2):
                wgb, wgk = cx.next_wb()
                tk.dma("pool", wgb[:], wgv[:, :, fb * 512:(fb + 1) * 512], w=[wgk])
                wub, wuk = cx.next_wb()
                tk.dma("pool", wub[:], wuv[:, :, fb * 512:(fb + 1) * 512], w=[wuk])
                out.append((wgb, wgk, wub, wuk))
            return out

        def load_wd(e_):
            wdv = wd[e_].rearrange("(kc p) f -> p kc f", p=128)
            for db in range(4):
                tk.dma("pool", wdsb[:, :, db * 512:(db + 1) * 512], wdv[:, :, db * 512:(db + 1) * 512], w=["wd%d" % db])

        gu = {0: load_gu(0)}
        load_wd(0)
        onesf = tk.sb([128, 128], F32)
        tk.op("dve", lambda e: e.memset(onesf[:], 1.0), w=["onesf"])
        ones64 = tk.sb([128, 64], F32)
        tk.op("dve", lambda e: e.memset(ones64[:], 1.0), w=["ones64"])
        mx = tk.sb([128, 64], F32)
        tk.op("dve", lambda e: e.tensor_reduce(out=mx[:], in_=lgs[:], axis=AX.X, op=ALU.max), r=["lgs"], w=["mx"])
        ex = tk.sb([128, 64, 16], F32)
        tk.op("dve", lambda e: e.tensor_tensor(out=ex[:], in0=lgs[:], in1=mx[:].unsqueeze(2).to_broadcast([128, 64, 16]),
                                               op=ALU.subtract), r=["lgs", "mx"], w=["ex0"])
        tk.op("act", lambda e: e.activation(out=ex[:], in_=ex[:], func=AF.Exp), r=["ex0"], w=["ex"])
        sm = tk.sb([128, 64], F32)
        tk.op("dve", lambda e: e.tensor_reduce(out=sm[:], in_=ex[:], axis=AX.X, op=ALU.add), r=["ex"], w=["sm0"])
        tk.op("dve", lambda e: e.reciprocal(out=sm[:], in_=sm[:]), r=["sm0"], w=["sm"])
        aff = tk.sb([128, 2, 64], F32)
        for e_ in range(2):
            tk.op("dve", lambda e, e_=e_: e.tensor_tensor(out=aff[:, e_, :], in0=ex[:, :, e_], in1=sm[:], op=ALU.mult),
                  r=["ex", "sm"], w=["aff%d" % e_])
        affk = ["aff0", "aff1"]
        lo = tk.sb([128, 2], F32)
        cand = tk.sb([128, 2], F32)
        cmp_ = tk.sb([128, 2, 64], F32)
        cnt = tk.sb([128, 2], F32)
        ge = tk.sb([128, 2], F32)
        tk.op("dve", lambda e: e.memset(lo[:], 0.0), w=["lo"])
        ps_b, pk_b = cx.next_ps()
        for it in range(BIS_ITERS):
            step = float(2.0 ** -(it + 1))
            tk.op("dve", lambda e: e.tensor_scalar(out=cand[:], in0=lo[:], scalar1=step, scalar2=None, op0=ALU.add),
                  r=["lo"], w=["cand"])
            tk.op("dve", lambda e: e.tensor_tensor(out=cmp_[:], in0=aff[:], in1=cand[:].unsqueeze(2).to_broadcast([128, 2, 64]),
                                                   op=ALU.is_ge), r=affk + ["cand"], w=["cmp"])
            tk.op("dve", lambda e: e.tensor_reduce(out=cnt[:], in_=cmp_[:], axis=AX.X, op=ALU.add), r=["cmp"], w=["cnt"])
            tk.op("pe", lambda e: e.matmul(ps_b[:, 0:2], onesf[:], cnt[:], start=True, stop=True), r=["cnt", "onesf"], w=[pk_b])
            tk.op("dve", lambda e: e.tensor_scalar(out=ge[:], in0=ps_b[:, 0:2], scalar1=float(CAP), scalar2=None, op0=ALU.is_ge),
                  r=[pk_b], w=["ge"])
            tk.op("dve", lambda e: e.scalar_tensor_tensor(out=lo[:], in0=ge[:], scalar=step, in1=lo[:], op0=ALU.mult, op1=ALU.add),
                  r=["ge", "lo"], w=["lo"])
        mask = tk.sb([128, 2, 64], F32)
        tk.op("dve", lambda e: e.tensor_tensor(out=mask[:], in0=aff[:], in1=lo[:].unsqueeze(2).to_broadcast([128, 2, 64]),
                                               op=ALU.is_ge), r=affk + ["lo"], w=["mask"])
        tk.op("dve", lambda e: e.tensor_reduce(out=cnt[:], in_=mask[:], axis=AX.X, op=ALU.add), r=["mask"], w=["cnt"])
        tk.op("pe", lambda e: e.matmul(ps_b[:, 0:2], lt[:], cnt[:], start=True, stop=True), r=["cnt", "lt"], w=[pk_b])
        offs = tk.sb([128, 2], F32)
        tk.op("dve", lambda e: e.tensor_copy(offs[:], ps_b[:, 0:2]), r=[pk_b], w=["offs"])
        cs = tk.sb([128, 2, 64], F32)
        pos = tk.sb([128, 2, 64], F32)
        seli = tk.sb([128, 2, 64], I32)
        tok = tk.sb([128, 64], I32)
        tk.op("pool", lambda e: e.iota(tok[:], pattern=[[1, 64]], base=0, channel_multiplier=64), w=["tok"])
        pay = tk.sb([128, 64, 2, 2], I32)
        payf = pay[:].bitcast(F32)
        invv = inv.rearrange("e (p j) -> e p j", j=64)
        for e_ in range(2):
            tk.op("dve", lambda e, e_=e_: e.tensor_tensor_scan(out=cs[:, e_, :], data0=ones64[:], data1=mask[:, e_, :], initial=0.0,
                                                               op0=ALU.mult, op1=ALU.add), r=["mask", "ones64"], w=["cs%d" % e_])
            tk.op("dve", lambda e, e_=e_: e.scalar_tensor_tensor(out=pos[:, e_, :], in0=cs[:, e_, :], scalar=offs[:, e_:e_ + 1],
                                                                 in1=mask[:, e_, :], op0=ALU.add, op1=ALU.subtract),
                  r=["cs%d" % e_, "offs", "mask"], w=["pos%d" % e_])
            tk.op("dve", lambda e, e_=e_: e.scalar_tensor_tensor(out=pos[:, e_, :], in0=pos[:, e_, :], scalar=-BIGSLOT,
                                                                 in1=mask[:, e_, :], op0=ALU.add, op1=ALU.mult),
                  r=["pos%d" % e_, "mask"], w=["pos%d" % e_])
            tk.op("dve", lambda e, e_=e_: e.tensor_scalar(out=seli[:, e_, :], in0=pos[:, e_, :], scalar1=BIGSLOT, scalar2=None,
                                                          op0=ALU.add), r=["pos%d" % e_], w=["seli%d" % e_])
            tk.dma("sp", invv[e_], seli[:, e_, :], r=["seli%d" % e_], is_output=True)
            tk.op("dve", lambda e, e_=e_: e.tensor_copy(pay[:, :, e_, 0], tok[:]), r=["tok"], w=["pay%d" % e_])
            tk.op("dve", lambda e, e_=e_: e.tensor_copy(payf[:, :, e_, 1], aff[:, e_, :]), r=[affk[e_], "pay%d" % e_], w=["pay%d" % e_])
        breg = nc.gpsimd.to_reg(CAP - 1)
        for e_ in range(2):
            for j in range(64):
                tk.idma(out=idxl[e_], out_off=bass.IndirectOffsetOnAxis(ap=seli[:, e_, j:j + 1], axis=0),
                        in_=pay[:, j, e_, :], in_off=None, r=["seli%d" % e_, "pay%d" % e_], wacc=["idxl%d" % e_],
                        bounds_check=breg, oob_is_err=False)
        idxsb = tk.sb([128, 2, 8, 2], I32)
        idxf = idxsb[:].bitcast(F32)
        for e_ in range(2):
            tk.dma("sp", idxsb[:, e_, :, :], idxl[e_][0:CAP, :].rearrange("(i p) w -> p i w", p=128), r=["idxl%d" % e_], w=["idxsb%d" % e_])
        xsT = tk.sb([128, KC, CAP], BF16, name="xsT")
        actT = tk.sb([128, 8, CAP], BF16, name="actT")
        xg = [tk.sb([128, D], BF16) for _ in range(2)]
        sa = [tk.sb([128, 512], F32) for _ in range(2)]
        ysb = [tk.sb([128, D], BF16) for _ in range(2)]
        gi = 0
        si = 0
        yi = 0
        for e_ in range(2):
            for i in range(8):
                g = xg[gi % 2]
                gk = "xg%d" % (gi % 2)
                gi += 1
                tk.idma(out=g[:], out_off=None, in_=h2, in_off=bass.IndirectOffsetOnAxis(ap=idxsb[:, e_, i, 0:1], axis=0),
                        r=["idxsb%d" % e_], w=[gk])
                for half in range(2):
                    pb, pbk = cx.next_psb()
                    for q in range(8):
                        kc = half * 8 + q
                        tk.op("pe", lambda e, kc=kc, q=q: e.transpose(pb[:, q * 128:(q + 1) * 128], g[:, kc * 128:(kc + 1) * 128], idb[:]),
                              r=[gk, "idb"], w=[pbk])
                    tk.op("act" if half == 0 else "dve",
                          lambda e, half=half: (e.activation(out=xsT[:, half * 8:half * 8 + 8, i * 128:(i + 1) * 128],
                                                             in_=pb[:].rearrange("p (q t) -> p q t", t=128), func=AF.Copy)
                                                if half == 0 else
                                                e.tensor_copy(xsT[:, half * 8:half * 8 + 8, i * 128:(i + 1) * 128],
                                                              pb[:].rearrange("p (q t) -> p q t", t=128))),
                          r=[pbk], w=["xsT_%d_%d" % (half, i)])
            xkeys = ["xsT_%d_%d" % (h_, i) for h_ in range(2) for i in range(8)]
            for fb in range(2):
                wgb, wgk, wub, wuk = gu[e_][fb]
                for fc in range(4):
                    f = fb * 4 + fc
                    for th in range(2):
                        pa, pak = cx.next_ps()
                        pbb, pbbk = cx.next_ps()
                        for kc in range(KC):
                            tk.op("pe", lambda e, kc=kc: e.matmul(pa[:, :], wgb[:, kc, fc * 128:(fc + 1) * 128],
                                                                  xsT[:, kc, th * 512:(th + 1) * 512], start=(kc == 0), stop=(kc == KC - 1)),
                                  r=[wgk] + xkeys, w=[pak])
                        for kc in range(KC):
                            tk.op("pe", lambda e, kc=kc: e.matmul(pbb[:, :], wub[:, kc, fc * 128:(fc + 1) * 128],
                                                                  xsT[:, kc, th * 512:(th + 1) * 512], start=(kc == 0), stop=(kc == KC - 1)),
                                  r=[wuk] + xkeys, w=[pbbk])
                        s_ = sa[si % 2]
                        sk = "sa%d" % (si % 2)
                        si += 1
                        tk.op("act", lambda e: e.activation(out=s_[:], in_=pa[:], func=AF.Silu), r=[pak], w=[sk])
                        tk.op("dve", lambda e: e.tensor_tensor(out=actT[:, f, th * 512:(th + 1) * 512], in0=s_[:], in1=pbb[:], op=ALU.mult),
                              r=[sk, pbbk], w=["actT%d_%d" % (f, th)])
            akeys = ["actT%d_%d" % (f, th) for f in range(8) for th in range(2)]
            if e_ == 0:
                gu[1] = load_gu(1)
            for i in range(8):
                y_ = ysb[yi % 2]
                yk = "ysb%d" % (yi % 2)
                yi += 1
                for db in range(4):
                    po, pok = cx.next_ps()
                    for f in range(8):
                        tk.op("pe", lambda e, f=f: e.matmul(po[:, :], actT[:, f, i * 128:(i + 1) * 128], wdsb[:, f, db * 512:(db + 1) * 512],
                                                            start=(f == 0), stop=(f == 7)), r=akeys + ["wd%d" % db], w=[pok])
                    tk.op("act" if db % 2 == 0 else "dve",
                          lambda e, db=db: (e.activation(out=y_[:, db * 512:(db + 1) * 512], in_=po[:], func=AF.Copy,
                                                         scale=idxf[:, e_, i, 1:2]) if db % 2 == 0 else
                                            e.tensor_scalar(out=y_[:, db * 512:(db + 1) * 512], in0=po[:], scalar1=idxf[:, e_, i, 1:2],
                                                            scalar2=None, op0=ALU.mult)),
                          r=[pok, "idxsb%d" % e_], w=[yk + "_%d" % db])
                tk.dma("sp", Y[e_, i * 128:(i + 1) * 128, :], y_[:], r=[yk + "_%d" % db for db in range(4)], is_output=True)
            if e_ == 0:
                load_wd(1)
        tk.finish()
    return nc


def moe_consts():
    lt = (np.arange(128)[:, None] < np.arange(128)[None, :]).astype(np.float32)
    return {"ltri": lt, "ident": np.eye(128, dtype=np.float32)}


def run_moe(h2T_full, lgT_full, inp, layer):
    nc = get_nc("moe", build_moe)
    h2 = np.ascontiguousarray(h2T_full.T)
    lg = np.ascontiguousarray(lgT_full.T)
    cst = moe_consts()
    maps = []
    for r in range(NCORES):
        perm = [2 * r, 2 * r + 1] + [e for e in range(16) if e not in (2 * r, 2 * r + 1)]
        m = {"lg": np.ascontiguousarray(lg[:, perm]), "h2": h2,
             "wg": np.ascontiguousarray(inp["moe_w_gate"][layer, 2 * r:2 * r + 2]),
             "wu": np.ascontiguousarray(inp["moe_w_up"][layer, 2 * r:2 * r + 2]),
             "wd": np.ascontiguousarray(inp["moe_w_down"][layer, 2 * r:2 * r + 2])}
        m.update(cst)
        maps.append(m)
    res = run(nc, maps)
    Y = np.concatenate([res[r]["Y"] for r in range(NCORES)], axis=0)
    inv = np.concatenate([res[r]["inv"] for r in range(NCORES)], axis=0)
    return Y, inv


def build_comb():
    nc = new_nc()
    x1 = din(nc, "x1", [T, D], F32)
    Ye = [din(nc, "Y%d" % e, [CAP, D], BF16) for e in range(16)]
    invp = din(nc, "invp", [128, 16 * 8], I32)
    gate = din(nc, "gate", [1, D], F32)
    ident = din(nc, "ident", [128, 128], F32)
    xo = dout(nc, "xo", [T, D], F32)
    es = ExitStack()
    with es:
        tk = TK(nc, es)
        psum = [tk.ps([128, 512], F32, name="psum%d" % i) for i in range(8)]
        invsb = tk.sb([128, 128], I32)
        tk.dma("sp", invsb[:], invp, w=["inv"])
        idb = tk.sb([128, 128], BF16)
        tk.dma("pool", idb[:], ident, w=["idb"])
        g1b = tk.sb([128, D], F32)
        tk.dma("sp", g1b[:], gate.to_broadcast([128, D]), w=["g1b0"])
        tk.op("dve", lambda e: e.tensor_scalar(out=g1b[:], in0=g1b[:], scalar1=1.0, scalar2=None, op0=ALU.add), r=["g1b0"], w=["g1b"])
        NZ = 8
        Z = [tk.sb([128, D], BF16) for _ in range(NZ)]
        acc = [tk.sb([128, D], F32) for _ in range(2)]
        xt = [tk.sb([128, D], F32) for _ in range(2)]
        breg = nc.gpsimd.to_reg(CAP - 1)
        zi = 0
        for i in range(8):
            a = acc[i % 2]
            ak = "acc%d" % (i % 2)
            x_ = xt[i % 2]
            xk = "xt%d" % (i % 2)
            tk.dma("sp", x_[:], x1[i * 128:(i + 1) * 128, :], w=[xk])
            pbank = [(psum[(i % 2) * 4 + c], "psum%d" % ((i % 2) * 4 + c)) for c in range(4)]
            for e_ in range(16):
                z = Z[zi % NZ]
                zk = "Z%d" % (zi % NZ)
                zi += 1
                tk.op("dve", lambda e, z=z: e.memset(z[:], 0.0), w=[zk])
                tk.idma(out=z[:], out_off=None, in_=Ye[e_], in_off=bass.IndirectOffsetOnAxis(ap=invsb[:, e_ * 8 + i:e_ * 8 + i + 1], axis=0),
                        r=["inv"], w=[zk], bounds_check=breg, oob_is_err=False)
                for c in range(4):
                    ps, pk = pbank[c]
                    tk.op("pe", lambda e, z=z, ps=ps, c=c, e_=e_: e.matmul(ps[:, :], idb[:], z[:, c * 512:(c + 1) * 512],
                                                                        start=(e_ == 0), stop=(e_ == 15)), r=[zk, "idb"], w=[pk])
            for c in range(4):
                ps, pk = pbank[c]
                cs = slice(c * 512, (c + 1) * 512)
                tk.op("dve", lambda e, ps=ps, cs=cs: e.tensor_tensor(out=a[:, cs], in0=ps[:, :], in1=g1b[:, cs], op=ALU.mult), r=[pk, "g1b"], w=[ak + "_%d" % c])
                tk.op("dve", lambda e, cs=cs: e.tensor_tensor(out=a[:, cs], in0=a[:, cs], in1=x_[:, cs], op=ALU.add), r=[ak + "_%d" % c, xk], w=[ak + "_%d" % c])
            tk.dma("sp", xo[i * 128:(i + 1) * 128, :], a[:], r=[ak + "_%d" % c for c in range(4)], is_output=True)
        tk.finish()
    return nc


def run_comb(x1T_full, Y, inv, gate_vec):
    nc = get_nc("comb", build_comb)
    maps = []
    for r in range(NCORES):
        iv = inv[:, r * T:(r + 1) * T].reshape(16, 8, 128)
        invp = np.ascontiguousarray(iv.transpose(2, 0, 1).reshape(128, 128))
        m = {"x1": np.ascontiguousarray(x1T_full[:, r * T:(r + 1) * T].T), "invp": invp,
             "gate": np.ascontiguousarray(gate_vec[None, :]), "ident": np.eye(128, dtype=np.float32)}
        for e in range(16):
            m["Y%d" % e] = Y[e]
        maps.append(m)
    res = run(nc, maps)
    return np.ascontiguousarray(np.concatenate([res[r]["xo"] for r in range(NCORES)], axis=0).T)


CONV_HALO = 15


def build_m_conv():
    nc = new_nc()
    halo = CONV_HALO
    TT = T + 2 * halo
    io = m_common_io(nc, halo)
    w_in = din(nc, "w_in", [D, 2 * D], F32)
    dwp = din(nc, "dw", [128, KC * 31], F32)
    lng = din(nc, "lng", [128, 16], F32)
    lnb = din(nc, "lnb", [128, 16], F32)
    w_out = din(nc, "w_out", [D, D], F32)
    vme = din(nc, "vme", [1, 2 * KC * halo], F32)
    es = ExitStack()
    with es:
        cx = Ctx(nc, es, n_wbuf=2)
        tk = cx.tk
        xs = load_xT(cx, io["xT"], TT)
        xk = lambda kc: "x%d" % kc
        modA = load_mod(cx, io["modA"], io["gA"])
        dws = tk.sb([128, KC, 31], F32)
        tk.dma("sp", dws[:], dwp.rearrange("p (kc k) -> p kc k", k=31), w=["dw"])
        lg_ = tk.sb([128, 16], F32)
        lb_ = tk.sb([128, 16], F32)
        tk.dma("sp", lg_[:], lng, w=["lng"])
        tk.dma("sp", lb_[:], lnb, w=["lnb"])
        vm = tk.sb([128, 2, KC, halo], F32)
        tk.dma("sp", vm[:], vme.rearrange("o (s kc h) -> o s kc h", s=2, kc=KC).to_broadcast([128, 2, KC, halo]), w=["vm"])
        eps1 = tk.sb([128, 1], F32)
        tk.op("dve", lambda e: e.memset(eps1[:], float(EPS)), w=["eps1"])
        bufA = tk.sb([128, KC, TT], BF16, name="bufA")
        bufB = tk.sb([128, KC, TT], BF16, name="bufB")
        kA = lambda kc: "A%d" % kc
        kB = lambda kc: "B%d" % kc
        rms_mod(cx, xs, xk, TT, 0, modA, bufA, kA)
        a_tmp = tk.sb([128, 4, TT], BF16)
        sg = cx.rm_tmp
        sgi = [0]
        for blk in range(4):
            def evac_a(ci, c0, M, tc0, n, ps, pkey, blk=blk):
                tk.op("act", lambda e: e.activation(out=a_tmp[:, ci, tc0:tc0 + n], in_=ps[:, 0:n], func=AF.Copy),
                      r=[pkey], w=["atmp%d_%d" % (ci, tc0)])

            def evac_b(ci, c0, M, tc0, n, ps, pkey, blk=blk):
                f = blk * 4 + ci
                s_ = sg[sgi[0] % 2]
                sk = "rmtmp%d" % (sgi[0] % 2)
                sgi[0] += 1
                tk.op("act", lambda e: e.activation(out=s_[:, 0:n], in_=ps[:, 0:n], func=AF.Sigmoid), r=[pkey], w=[sk])
                tk.op("dve", lambda e: e.tensor_tensor(out=bufB[:, f, tc0:tc0 + n], in0=s_[:, 0:n], in1=a_tmp[:, ci, tc0:tc0 + n], op=ALU.mult),
                      r=[sk, "atmp%d_%d" % (ci, tc0)], w=[kB(f)])
            linear_fm(cx, w_in, bufA, kA, KC, [((blk * 4 + j) * 128, 128) for j in range(4)], TT, evac_a)
            linear_fm(cx, w_in, bufA, kA, KC, [(D + (blk * 4 + j) * 128, 128) for j in range(4)], TT, evac_b)
        allB = [kB(f) for f in range(KC)]
        tk.op("dve", lambda e: e.tensor_tensor(out=bufB[:, :, 0:halo], in0=bufB[:, :, 0:halo], in1=vm[:, 0, :, :], op=ALU.mult),
              r=allB + ["vm"], w=allB)
        tk.op("dve", lambda e: e.tensor_tensor(out=bufB[:, :, TT - halo:TT], in0=bufB[:, :, TT - halo:TT], in1=vm[:, 1, :, :], op=ALU.mult),
              r=allB + ["vm"], w=allB)
        for f in range(KC):
            c = xs[:, f, 0:T]
            tk.op("dve", lambda e: e.tensor_scalar(out=c, in0=bufB[:, f, 0:T], scalar1=dws[:, f, 0:1], scalar2=None, op0=ALU.mult),
                  r=[kB(f), "dw"], w=[xk(f)])
            for k in range(1, 31):
                tk.op("dve", lambda e, k=k: e.scalar_tensor_tensor(out=c, in0=bufB[:, f, k:k + T], scalar=dws[:, f, k:k + 1], in1=c,
                                                                   op0=ALU.mult, op1=ALU.add), r=[kB(f), "dw", xk(f)], w=[xk(f)])
        mu = tk.sb([128, T], F32)
        rstd = tk.sb([128, T], F32)
        msq = cx.rm_hf[0]
        cb = [tk.sb([128, 512], BF16) for _ in range(2)]
        cq = [tk.sb([128, 512], BF16) for _ in range(2)]
        for ti, (tc0, n) in enumerate(tchunks(T)):
            p1, k1 = cx.next_ps()
            p2, k2 = cx.next_ps()
            for f in range(KC):
                b1, b1k = cb[f % 2], "cb%d" % (f % 2)
                b2, b2k = cq[f % 2], "cq%d" % (f % 2)
                tk.op("act", lambda e: e.activation(out=b1[:, 0:n], in_=xs[:, f, tc0:tc0 + n], func=AF.Copy), r=[xk(f)], w=[b1k])
                tk.op("act", lambda e: e.activation(out=b2[:, 0:n], in_=xs[:, f, tc0:tc0 + n], func=AF.Square), r=[xk(f)], w=[b2k])
                tk.op("pe", lambda e: e.matmul(p1[:, 0:n], cx.ones[:], b1[:, 0:n], start=(f == 0), stop=(f == KC - 1)), r=[b1k, "ones"], w=[k1])
                tk.op("pe", lambda e: e.matmul(p2[:, 0:n], cx.ones[:], b2[:, 0:n], start=(f == 0), stop=(f == KC - 1)), r=[b2k, "ones"], w=[k2])
            tk.op("dve", lambda e: e.tensor_scalar(out=mu[:, tc0:tc0 + n], in0=p1[:, 0:n], scalar1=1.0 / D, scalar2=None, op0=ALU.mult),
                  r=[k1], w=["mu%d" % ti])
            tk.op("dve", lambda e: e.tensor_tensor(out=msq[:, 0:n], in0=mu[:, tc0:tc0 + n], in1=mu[:, tc0:tc0 + n], op=ALU.mult),
                  r=["mu%d" % ti], w=["rmhf0"])
            tk.op("dve", lambda e: e.scalar_tensor_tensor(out=rstd[:, tc0:tc0 + n], in0=p2[:, 0:n], scalar=1.0 / D, in1=msq[:, 0:n],
                                                          op0=ALU.mult, op1=ALU.subtract), r=[k2, "rmhf0"], w=["rstd%d" % ti])
            tk.op("act", lambda e: e.activation(out=rstd[:, tc0:tc0 + n], in_=rstd[:, tc0:tc0 + n], func=AF.Sqrt, bias=eps1[:, 0:1], scale=1.0),
                  r=["rstd%d" % ti, "eps1"], w=["rstd%d" % ti])
            tk.op("dve", lambda e: e.reciprocal(out=rstd[:, tc0:tc0 + n], in_=rstd[:, tc0:tc0 + n]), r=["rstd%d" % ti], w=["rstd%d" % ti])
        stat_keys = ["mu0", "mu1", "rstd0", "rstd1"]
        for f in range(KC):
            c = xs[:, f, 0:T]
            tk.op("dve", lambda e: e.tensor_tensor(out=c, in0=c, in1=mu[:], op=ALU.subtract), r=[xk(f)] + stat_keys, w=[xk(f)])
            tk.op("dve", lambda e: e.tensor_tensor(out=c, in0=c, in1=rstd[:], op=ALU.mult), r=[xk(f)] + stat_keys, w=[xk(f)])
            tk.op("act", lambda e: e.activation(out=bufA[:, f, 0:T], in_=c, func=AF.Silu, bias=lb_[:, f:f + 1], scale=lg_[:, f:f + 1]),
                  r=[xk(f), "lng", "lnb"], w=[kA(f)])
        xv = io["xT"].rearrange("(kc p) t -> p kc t", p=128)
        for f in range(KC):
            tk.dma("sp", xs[:, f, halo:halo + T], xv[:, f, halo:halo + T], w=[xk(f)])
        epilogue(cx, xs, halo, bufA, kA, KC, w_out, modA, io["modB"], io["gB"], io["router"], io, bufB, kB)
        tk.finish()
    return nc


def run_m_conv(xT_full, inp, mod, layer):
    nc = get_nc("m_conv", build_m_conv)
    halo = CONV_HALO
    maps = common_maps(xT_full, inp, mod, layer, halo)
    dw = inp["conv_dw"][0]
    dwp = np.ascontiguousarray(dw.reshape(31, KC, 128).transpose(2, 1, 0).reshape(128, KC * 31))
    for r in range(NCORES):
        lo = r * T - halo
        left = ((np.arange(lo, lo + halo) >= 0)).astype(np.float32)
        right = ((np.arange((r + 1) * T, (r + 1) * T + halo) < S)).astype(np.float32)
        vme = np.concatenate([np.tile(left, KC), np.tile(right, KC)])[None, :]
        maps[r].update({
            "w_in": np.ascontiguousarray(inp["conv_w_in"][0]), "dw": dwp,
            "lng": fm16(inp["conv_ln_g"][0]), "lnb": fm16(inp["conv_ln_b"][0]),
            "w_out": np.ascontiguousarray(inp["conv_w_out"][0]), "vme": np.ascontiguousarray(vme),
        })
    return gather_common(run(nc, maps))


TWO_PI_HI = 6.28125
TWO_PI_LO = 2.0 * np.pi - 6.28125
PI_SAFE = 3.1415925


def rope_tables(cx, pos_ap, inv_ap, npart, n):
    tk = cx.tk
    pi_ = tk.sb([npart, n], I32)
    ang = tk.sb([npart, n], F32)
    ki = tk.sb([npart, n], I32)
    kf = tk.sb([npart, n], F32)
    inv = tk.sb([npart, 1], F32)
    cos = tk.sb([npart, n], F32, name="rope_cos")
    sin = tk.sb([npart, n], F32, name="rope_sin")
    tk.dma("sp", pi_[:], pos_ap.to_broadcast([npart, n]), w=["rp_pi"])
    tk.dma("sp", inv[:], inv_ap, w=["rp_inv"])
    tk.op("dve", lambda e: e.tensor_copy(ang[:], pi_[:]), r=["rp_pi"], w=["rp_ang"])
    tk.op("dve", lambda e: e.tensor_scalar(out=ang[:], in0=ang[:], scalar1=inv[:, 0:1], scalar2=None, op0=ALU.mult),
          r=["rp_ang", "rp_inv"], w=["rp_ang"])
    for (dst, shift, key) in ((sin, 0.0, "rope_sin"), (cos, float(np.pi / 2), "rope_cos")):
        if shift != 0.0:
            tk.op("dve", lambda e: e.tensor_scalar(out=dst[:], in0=ang[:], scalar1=shift, scalar2=None, op0=ALU.add), r=["rp_ang"], w=[key])
            src, sk = dst, key
        else:
            src, sk = ang, "rp_ang"
        tk.op("dve", lambda e, src=src: e.tensor_scalar(out=ki[:], in0=src[:], scalar1=float(1.0 / (2 * np.pi)), scalar2=None, op0=ALU.mult),
              r=[sk], w=["rp_ki"])
        tk.op("dve", lambda e: e.tensor_copy(kf[:], ki[:]), r=["rp_ki"], w=["rp_kf"])
        tk.op("dve", lambda e, src=src, dst=dst: e.scalar_tensor_tensor(out=dst[:], in0=kf[:], scalar=-TWO_PI_HI, in1=src[:], op0=ALU.mult, op1=ALU.add),
              r=["rp_kf", sk], w=[key])
        tk.op("dve", lambda e, dst=dst: e.scalar_tensor_tensor(out=dst[:], in0=kf[:], scalar=-TWO_PI_LO, in1=dst[:], op0=ALU.mult, op1=ALU.add),
              r=["rp_kf", key], w=[key])
        tk.op("dve", lambda e, dst=dst: e.tensor_scalar(out=dst[:], in0=dst[:], scalar1=PI_SAFE, scalar2=-PI_SAFE, op0=ALU.min, op1=ALU.max),
              r=[key], w=[key])
        tk.op("act", lambda e, dst=dst: e.activation(out=dst[:], in_=dst[:], func=AF.Sin), r=[key], w=[key])
    return cos, sin


def rot_matrix(half):
    n = 2 * half
    R = np.zeros((n, n), np.float32)
    for m in range(half):
        R[m + half, m] = -1.0
        R[m, m + half] = 1.0
    return R


def inv_freq(half, npart):
    inv = 10000.0 ** (-np.arange(half, dtype=np.float32) / half)
    return np.ascontiguousarray(np.tile(inv, npart // half)[:, None].astype(np.float32))


def build_ma_dil():
    nc = new_nc()
    xT = din(nc, "xT", [D, T], F32)
    modA_ap = din(nc, "modA", [128, 48], F32)
    gA = din(nc, "gA", [128, 16], F32)
    w_in = din(nc, "w_in", [D, 9216], F32)
    pos = din(nc, "pos", [1, T], I32)
    invf = din(nc, "invf", [128, 1], F32)
    rm_ap = din(nc, "rotm", [128, 128], F32)
    gq_ap = din(nc, "gq", [128, 3], F32)
    gk_ap = din(nc, "gk", [128, 3], F32)
    qkv = dout(nc, "qkv", [72, 128, T], BF16)
    es = ExitStack()
    with es:
        cx = Ctx(nc, es, n_wbuf=3, n_ps=8, n_psb=0)
        tk = cx.tk
        xs = load_xT(cx, xT, T)
        modA = load_mod(cx, modA_ap, gA)
        cos, sin = rope_tables(cx, pos, invf, 128, T)
        rmb = tk.sb([128, 128], BF16)
        tk.dma("pool", rmb[:], rm_ap, w=["rmb"])
        gq = tk.sb([128, 3], F32)
        gk = tk.sb([128, 3], F32)
        tk.dma("sp", gq[:], gq_ap, w=["gq"])
        tk.dma("sp", gk[:], gk_ap, w=["gk"])
        eps1 = tk.sb([128, 1], F32)
        tk.op("dve", lambda e: e.memset(eps1[:], float(EPS)), w=["eps1"])
        hT = tk.sb([128, KC, T], BF16, name="hT")
        kA = lambda kc: "A%d" % kc
        rms_mod(cx, xs, lambda kc: "x%d" % kc, T, 0, modA, hT, kA)
        qg = [tk.sb([128, 512], F32) for _ in range(2)]
        qgb = [tk.sb([128, 512], BF16) for _ in range(2)]
        t1 = [tk.sb([128, 512], F32) for _ in range(2)]
        ob = [tk.sb([128, 512], BF16) for _ in range(3)]
        cnt = [0, 0]

        def evac(ci, c0, M, tc0, n, ps, pkey):
            g = ci // 24
            kind = (ci // 8) % 3
            o_ = ob[cnt[1] % 3]
            ok = "ob%d" % (cnt[1] % 3)
            cnt[1] += 1
            if kind == 2:
                tk.op("act", lambda e: e.activation(out=o_[:, 0:n], in_=ps[:, 0:n], func=AF.Copy), r=[pkey], w=[ok])
            else:
                i = cnt[0] % 2
                cnt[0] += 1
                gain = gq if kind == 0 else gk
                gkey = "gq" if kind == 0 else "gk"
                sq, sqk = cx.rm_sq[i], "rmsq%d" % i
                tk.op("act", lambda e: e.activation(out=sq[:, 0:n], in_=ps[:, 0:n], func=AF.Square), r=[pkey], w=[sqk])
                pn, pnk = cx.next_ps()
                tk.op("pe", lambda e: e.matmul(pn[:, 0:n], cx.ones[:], sq[:, 0:n], start=True, stop=True), r=[sqk, "ones"], w=[pnk])
                rs, rsk = cx.rm_tmp[i], "rmtmp%d" % i
                tk.op("act", lambda e: e.activation(out=rs[:, 0:n], in_=pn[:, 0:n], func=AF.Sqrt, bias=eps1[:, 0:1], scale=1.0 / 128),
                      r=[pnk, "eps1"], w=[rsk])
                tk.op("dve", lambda e: e.reciprocal(out=rs[:, 0:n], in_=rs[:, 0:n]), r=[rsk], w=[rsk])
                q_, qk = qg[i], "qg%d" % i
                tk.op("dve", lambda e: e.scalar_tensor_tensor(out=q_[:, 0:n], in0=ps[:, 0:n], scalar=gain[:, g:g + 1], in1=rs[:, 0:n],
                                                              op0=ALU.mult, op1=ALU.mult), r=[pkey, gkey, rsk], w=[qk])
                qb_, qbk = qgb[i], "qgb%d" % i
                tk.op("act", lambda e: e.activation(out=qb_[:, 0:n], in_=q_[:, 0:n], func=AF.Copy), r=[qk], w=[qbk])
                pr, prk = cx.next_ps()
                tk.op("pe", lambda e: e.matmul(pr[:, 0:n], rmb[:], qb_[:, 0:n], start=True, stop=True), r=[qbk, "rmb"], w=[prk])
                t_, tkk = t1[i], "t1_%d" % i
                tk.op("dve", lambda e: e.tensor_tensor(out=t_[:, 0:n], in0=q_[:, 0:n], in1=cos[:, tc0:tc0 + n], op=ALU.mult),
                      r=[qk, "rope_cos"], w=[tkk])
                tk.op("dve", lambda e: e.tensor_tensor(out=q_[:, 0:n], in0=pr[:, 0:n], in1=sin[:, tc0:tc0 + n], op=ALU.mult),
                      r=[prk, "rope_sin", qk, qbk], w=[qk])
                tk.op("dve", lambda e: e.tensor_tensor(out=o_[:, 0:n], in0=t_[:, 0:n], in1=q_[:, 0:n], op=ALU.add), r=[tkk, qk], w=[ok])
            tk.dma("sp", qkv[ci, :, tc0:tc0 + n], o_[:, 0:n], r=[ok], is_output=True)

        linear_fm(cx, w_in, hT, kA, KC, [(c * 128, 128) for c in range(72)], T, evac)
        tk.finish()
    return nc


DIL = ((128, 1), (512, 4), (2048, 16))


def build_mb_dil():
    nc = new_nc()
    Qs, Ks, Vs, VMs = [], [], [], []
    for g, (w, d) in enumerate(DIL):
        L = S // d
        Qs.append(din(nc, "Q%d" % g, [128, S], BF16))
        Ks.append(din(nc, "K%d" % g, [128, d * (L + 128)], BF16))
        Vs.append(din(nc, "V%d" % g, [d * (L + 128), 128], BF16))
        VMs.append(din(nc, "VM%d" % g, [d * (L + 128), 128], BF16))
    band_ap = din(nc, "band", [128, 256], F32)
    oT = dout(nc, "oT", [128, S], BF16)
    es = ExitStack()
    with es:
        cx = Ctx(nc, es, n_wbuf=0, n_ps=8, n_psb=0)
        tk = cx.tk
        band = tk.sb([128, 256], BF16)
        tk.dma("pool", band[:], band_ap, w=["band"])
        numer = tk.sb([128, S], F32, name="numer")
        den = tk.sb([128, S], F32, name="den")
        Qsb = tk.sb([128, S], BF16, name="Qsb")
        Ksb = tk.sb([128, 16 * (512 + 128)], BF16, name="Ksb")
        Vsb = tk.sb([128, 80, 128], BF16, name="Vsb")
        VMsb = tk.sb([128, 80, 128], BF16, name="VMsb")
        pe_ = [tk.sb([128, 256], BF16) for _ in range(4)]
        pm_ = [tk.sb([128, 256], BF16) for _ in range(4)]
        scale = float(128 ** -0.5)
        ui = 0
        for g, (w, d) in enumerate(DIL):
            L = S // d
            Lh = L + 128
            ntile = d * Lh // 128
            tk.dma("sp", Qsb[:], Qs[g], w=["Q"])
            tk.dma("sp", Ksb[:, 0:d * Lh], Ks[g], w=["K"])
            Vv = Vs[g].rearrange("(n p) c -> p n c", p=128)
            VMv = VMs[g].rearrange("(n p) c -> p n c", p=128)
            for a in range(0, ntile, 16):
                b = min(ntile, a + 16)
                tk.dma("sp", Vsb[:, a:b, :], Vv[:, a:b, :], w=["V%d" % (a // 16)])
                tk.dma("sp", VMsb[:, a:b, :], VMv[:, a:b, :], w=["VM%d" % (a // 16)])
            vkeys = ["V%d" % i for i in range(5)] + ["VM%d" % i for i in range(5)]
            units = [(p, qt) for p in range(d) for qt in range(L // 128)]
            LA = 2
            pend = {}
            for step in range(len(units) + LA):
                if step < len(units):
                    p, qt = units[step]
                    i3 = ui % 4
                    ui += 1
                    ps_s, ks_ = cx.next_ps()
                    qcol = p * L + qt * 128
                    for j in range(2):
                        kcol = p * Lh + (qt + j) * 128
                        tk.op("pe", lambda e, j=j, kcol=kcol, ps_s=ps_s, qcol=qcol: e.matmul(ps_s[:, j * 128:(j + 1) * 128], Ksb[:, kcol:kcol + 128],
                                                                                            Qsb[:, qcol:qcol + 128], start=True, stop=True),
                              r=["Q", "K"], w=[ks_])
                    pe, pek = pe_[i3], "pe%d" % i3
                    tk.op("act", lambda e, pe=pe, ps_s=ps_s: e.activation(out=pe[:], in_=ps_s[:, 0:256], func=AF.Exp, scale=scale), r=[ks_], w=[pek])
                    pm, pmk = pm_[i3], "pm%d" % i3
                    tk.op("pool", lambda e, pm=pm, pe=pe: e.tensor_tensor(out=pm[:], in0=pe[:], in1=band[:], op=ALU.mult), r=[pek, "band"], w=[pmk])
                    pend[step] = (pm, pmk)
                if step >= LA:
                    p, qt = units[step - LA]
                    pm, pmk = pend.pop(step - LA)
                    ps_o, ko_ = cx.next_ps()
                    ps_d, kd_ = cx.next_ps()
                    for j in range(2):
                        tile = (p * Lh) // 128 + qt + j
                        tk.op("pe", lambda e, j=j, tile=tile, pm=pm, ps_o=ps_o: e.matmul(ps_o[:, 0:128], Vsb[:, tile, :], pm[:, j * 128:(j + 1) * 128],
                                                                                        start=(j == 0), stop=(j == 1)), r=[pmk] + vkeys, w=[ko_])
                    for j in range(2):
                        tile = (p * Lh) // 128 + qt + j
                        tk.op("pe", lambda e, j=j, tile=tile, pm=pm, ps_d=ps_d: e.matmul(ps_d[:, 0:128], VMsb[:, tile, :], pm[:, j * 128:(j + 1) * 128],
                                                                                        start=(j == 0), stop=(j == 1)), r=[pmk] + vkeys, w=[kd_])
                    t0 = qt * 128 * d + p
                    nsl = numer[:, t0:t0 + 127 * d + 1:d] if d > 1 else numer[:, t0:t0 + 128]
                    dsl = den[:, t0:t0 + 127 * d + 1:d] if d > 1 else den[:, t0:t0 + 128]
                    blk = "nd%d" % ((qt * 128 * d) // 2048)
                    if g == 0:
                        tk.op("dve", lambda e, nsl=nsl, ps_o=ps_o: e.tensor_copy(nsl, ps_o[:, 0:128]), r=[ko_], w=[blk + "n"])
                        tk.op("dve", lambda e, dsl=dsl, ps_d=ps_d: e.tensor_copy(dsl, ps_d[:, 0:128]), r=[kd_], w=[blk + "d"])
                    else:
                        tk.op("dve", lambda e, nsl=nsl, ps_o=ps_o: e.tensor_tensor(out=nsl, in0=nsl, in1=ps_o[:, 0:128], op=ALU.add), r=[ko_, blk + "n"], w=[blk + "n"])
                        tk.op("dve", lambda e, dsl=dsl, ps_d=ps_d: e.tensor_tensor(out=dsl, in0=dsl, in1=ps_d[:, 0:128], op=ALU.add), r=[kd_, blk + "d"], w=[blk + "d"])
        ob = [tk.sb([128, 512], BF16) for _ in range(2)]
        for c in range(S // 512):
            blk = "nd%d" % ((c * 512) // 2048)
            o_, ok = ob[c % 2], "ob%d" % (c % 2)
            tk.op("dve", lambda e: e.reciprocal(out=den[:, c * 512:(c + 1) * 512], in_=den[:, c * 512:(c + 1) * 512]), r=[blk + "d"], w=[blk + "d"])
            tk.op("dve", lambda e: e.tensor_tensor(out=o_[:], in0=numer[:, c * 512:(c + 1) * 512], in1=den[:, c * 512:(c + 1) * 512], op=ALU.mult),
                  r=[blk + "n", blk + "d"], w=[ok])
            tk.dma("sp", oT[:, c * 512:(c + 1) * 512], o_[:], r=[ok], is_output=True)
        tk.finish()
    return nc


def build_mc(nk):
    nc = new_nc()
    io = m_common_io(nc, 0)
    oT = din(nc, "oT", [nk * 128, T], BF16)
    w_out = din(nc, "w_out", [nk * 128, D], F32)
    es = ExitStack()
    with es:
        cx = Ctx(nc, es, n_wbuf=3)
        tk = cx.tk
        xs = load_xT(cx, io["xT"], T)
        modA = load_mod(cx, io["modA"], io["gA"])
        bufA = tk.sb([128, KC, T], BF16, name="bufA")
        bufB = tk.sb([128, KC, T], BF16, name="bufB")
        kA = lambda kc: "A%d" % kc
        kB = lambda kc: "B%d" % kc
        ov = oT.rearrange("(kc p) t -> p kc t", p=128)
        for kc in range(nk):
            tk.dma("sp", bufA[:, kc, :], ov[:, kc, :], w=[kA(kc)])
        epilogue(cx, xs, 0, bufA, kA, nk, w_out, modA, io["modB"], io["gB"], io["router"], io, bufB, kB)
        tk.finish()
    return nc


def run_dil_layer(xT_full, inp, mod, layer, positions):
    nc = get_nc("ma_dil", build_ma_dil)
    maps = []
    for r in range(NCORES):
        maps.append({
            "xT": np.ascontiguousarray(xT_full[:, r * T:(r + 1) * T]),
            "modA": mod48(mod[layer, 0]), "gA": fm16(inp["norm_g"][layer, 0]),
            "w_in": np.ascontiguousarray(inp["dil_w_in"][0]),
            "pos": np.ascontiguousarray(positions[:, r * T:(r + 1) * T]).astype(np.int32),
            "invf": inv_freq(64, 128), "rotm": rot_matrix(64),
            "gq": np.ascontiguousarray(inp["dil_q_norm"][0].T), "gk": np.ascontiguousarray(inp["dil_k_norm"][0].T),
        })
    res = run(nc, maps)
    qkv = np.concatenate([res[r]["qkv"] for r in range(NCORES)], axis=2)
    nc = get_nc("mb_dil", build_mb_dil)
    kk = np.arange(128)[:, None]
    ii = np.arange(128)[None, :]
    band = np.concatenate([(np.abs(ii + 64 - 128 * j - kk) <= 64).astype(np.float32) for j in range(2)], axis=1)
    maps = []
    for h in range(NCORES):
        m = {"band": band}
        for g, (w, d) in enumerate(DIL):
            L = S // d
            q = qkv[(g * 3 + 0) * 8 + h]
            k = qkv[(g * 3 + 1) * 8 + h]
            v = qkv[(g * 3 + 2) * 8 + h]
            m["Q%d" % g] = np.ascontiguousarray(q.reshape(128, L, d).transpose(0, 2, 1).reshape(128, S))
            kp = np.zeros((128, d, L + 128), q.dtype)
            kp[:, :, 64:64 + L] = k.reshape(128, L, d).transpose(0, 2, 1)
            m["K%d" % g] = kp.reshape(128, d * (L + 128))
            vp = np.zeros((d, L + 128, 128), q.dtype)
            vp[:, 64:64 + L, :] = v.T.reshape(L, d, 128).transpose(1, 0, 2)
            m["V%d" % g] = vp.reshape(d * (L + 128), 128)
            vm = np.zeros((d, L + 128, 128), q.dtype)
            vm[:, 64:64 + L, :] = 1
            m["VM%d" % g] = vm.reshape(d * (L + 128), 128)
        maps.append(m)
    res = run(nc, maps)
    oT = np.concatenate([res[h]["oT"] for h in range(NCORES)], axis=0)
    return run_mc(xT_full, oT, inp["dil_w_out"][0], inp, mod, layer, 8)


def run_mc(xT_full, oT, w_out, inp, mod, layer, nk):
    nc = get_nc("mc%d" % nk, lambda: build_mc(nk))
    maps = common_maps(xT_full, inp, mod, layer, 0)
    for r in range(NCORES):
        maps[r].update({"oT": np.ascontiguousarray(oT[:, r * T:(r + 1) * T]), "w_out": np.ascontiguousarray(w_out)})
    return gather_common(run(nc, maps))


def build_ma_mla():
    nc = new_nc()
    xT = din(nc, "xT", [D, T], F32)
    modA_ap = din(nc, "modA", [128, 48], F32)
    gA = din(nc, "gA", [128, 16], F32)
    w_in = din(nc, "w_in", [D, 1088], F32)
    w_q_up = din(nc, "w_q_up", [512, 3072], F32)
    w_kv_up = din(nc, "w_kv_up", [512, 4096], F32)
    pos = din(nc, "pos", [1, T], I32)
    invf = din(nc, "invf", [64, 1], F32)
    rm_ap = din(nc, "rotm", [64, 64], F32)
    ga_ap = din(nc, "ga", [128, 8], F32)
    gqk_ap = din(nc, "gqk", [128, 4], F32)
    QnT = dout(nc, "QnT", [16, 128, T], BF16)
    QrT = dout(nc, "QrT", [16, 64, T], BF16)
    KnT = dout(nc, "KnT", [16, 128, T], BF16)
    KrT = dout(nc, "KrT", [16, 64, T], BF16)
    VT = dout(nc, "VT", [16, 128, T], BF16)
    es = ExitStack()
    with es:
        cx = Ctx(nc, es, n_wbuf=2, n_ps=8, n_psb=0)
        tk = cx.tk
        xs = load_xT(cx, xT, T)
        modA = load_mod(cx, modA_ap, gA)
        cos, sin = rope_tables(cx, pos, invf, 64, T)
        rmb = tk.sb([64, 64], BF16)
        tk.dma("pool", rmb[:], rm_ap, w=["rmb"])
        ga = tk.sb([128, 8], F32)
        gqk = tk.sb([128, 4], F32)
        tk.dma("sp", ga[:], ga_ap, w=["ga"])
        tk.dma("sp", gqk[:], gqk_ap, w=["gqk"])
        eps1 = tk.sb([128, 1], F32)
        tk.op("dve", lambda e: e.memset(eps1[:], float(EPS)), w=["eps1"])
        bufA = tk.sb([128, KC, T], BF16, name="bufA")
        kA = lambda kc: "A%d" % kc
        rms_mod(cx, xs, lambda kc: "x%d" % kc, T, 0, modA, bufA, kA)
        cf = xs
        ck = lambda j: "x%d" % j

        def evac_lat(ci, c0, M, tc0, n, ps, pkey):
            tk.op("act" if ci % 2 == 0 else "dve",
                  (lambda e: e.activation(out=cf[0:M, ci, tc0:tc0 + n], in_=ps[0:M, 0:n], func=AF.Copy)) if ci % 2 == 0 else
                  (lambda e: e.tensor_copy(cf[0:M, ci, tc0:tc0 + n], ps[0:M, 0:n])), r=[pkey], w=[ck(ci)])
        linear_fm(cx, w_in, bufA, kA, KC, [(j * 128, 128) for j in range(8)] + [(1024, 64)], T, evac_lat)
        bufB = tk.sb([128, 8, T], BF16, name="bufB")
        kB = lambda j: "B%d" % j
        for grp in range(2):
            for ti, (tc0, n) in enumerate(tchunks(T)):
                pn, pnk = cx.next_ps()
                for j in range(4):
                    sq, sqk = cx.rm_sq[j % 2], "rmsq%d" % (j % 2)
                    tk.op("act", lambda e: e.activation(out=sq[:, 0:n], in_=cf[:, grp * 4 + j, tc0:tc0 + n], func=AF.Square),
                          r=[ck(grp * 4 + j)], w=[sqk])
                    tk.op("pe", lambda e: e.matmul(pn[:, 0:n], cx.ones[:], sq[:, 0:n], start=(j == 0), stop=(j == 3)), r=[sqk, "ones"], w=[pnk])
                rs, rsk = cx.rm_rstd, "rmrstd"
                tk.op("act", lambda e: e.activation(out=rs[:, 0:n], in_=pn[:, 0:n], func=AF.Sqrt, bias=eps1[:, 0:1], scale=1.0 / 512),
                      r=[pnk, "eps1"], w=[rsk])
                tk.op("dve", lambda e: e.reciprocal(out=rs[:, 0:n], in_=rs[:, 0:n]), r=[rsk], w=[rsk])
                for j in range(4):
                    jj = grp * 4 + j
                    tk.op("dve", lambda e, jj=jj: e.scalar_tensor_tensor(out=bufB[:, jj, tc0:tc0 + n], in0=cf[:, jj, tc0:tc0 + n],
                                                                         scalar=ga[:, jj:jj + 1], in1=rs[:, 0:n], op0=ALU.mult, op1=ALU.mult),
                          r=[ck(jj), "ga", rsk], w=[kB(jj)])
        for ti, (tc0, n) in enumerate(tchunks(T)):
            sq, sqk = cx.rm_sq[ti % 2], "rmsq%d" % (ti % 2)
            tk.op("act", lambda e: e.activation(out=sq[0:64, 0:n], in_=cf[0:64, 8, tc0:tc0 + n], func=AF.Square), r=[ck(8)], w=[sqk])
            pn, pnk = cx.next_ps()
            tk.op("pe", lambda e: e.matmul(pn[:, 0:n], cx.ones[0:64, :], sq[0:64, 0:n], start=True, stop=True), r=[sqk, "ones"], w=[pnk])
            tk.op("dve", lambda e: e.tensor_copy(cf[:, 9, tc0:tc0 + n], pn[:, 0:n]), r=[pnk], w=[ck(9)])

        qg = [tk.sb([64, 512], F32) for _ in range(2)]
        qgb = [tk.sb([64, 512], BF16) for _ in range(2)]
        t1 = [tk.sb([64, 512], F32) for _ in range(2)]
        ob = [tk.sb([128, 512], BF16) for _ in range(4)]
        cnt = [0, 0]

        def nxt_ob():
            o_ = ob[cnt[1] % 4]
            ok = "ob%d" % (cnt[1] % 4)
            cnt[1] += 1
            return o_, ok

        def rope64(src_ap, src_keys, gcol, rs, rsk, tc0, n, dst_dram):
            i = cnt[0] % 2
            cnt[0] += 1
            q_, qk = qg[i], "qg%d" % i
            tk.op("dve", lambda e: e.scalar_tensor_tensor(out=q_[:, 0:n], in0=src_ap, scalar=gqk[0:64, gcol:gcol + 1], in1=rs[0:64, 0:n],
                                                          op0=ALU.mult, op1=ALU.mult), r=src_keys + ["gqk", rsk], w=[qk])
            qb_, qbk = qgb[i], "qgb%d" % i
            tk.op("act", lambda e: e.activation(out=qb_[:, 0:n], in_=q_[:, 0:n], func=AF.Copy), r=[qk], w=[qbk])
            pr, prk = cx.next_ps()
            tk.op("pe", lambda e: e.matmul(pr[0:64, 0:n], rmb[:], qb_[:, 0:n], start=True, stop=True), r=[qbk, "rmb"], w=[prk])
            t_, tkk = t1[i], "t1_%d" % i
            tk.op("dve", lambda e: e.tensor_tensor(out=t_[:, 0:n], in0=q_[:, 0:n], in1=cos[:, tc0:tc0 + n], op=ALU.mult), r=[qk, "rope_cos"], w=[tkk])
            tk.op("dve", lambda e: e.tensor_tensor(out=q_[:, 0:n], in0=pr[0:64, 0:n], in1=sin[:, tc0:tc0 + n], op=ALU.mult),
                  r=[prk, "rope_sin", qk, qbk], w=[qk])
            o_, ok = nxt_ob()
            tk.op("dve", lambda e: e.tensor_tensor(out=o_[0:64, 0:n], in0=t_[:, 0:n], in1=q_[:, 0:n], op=ALU.add), r=[tkk, qk], w=[ok])
            tk.dma("sp", dst_dram, o_[0:64, 0:n], r=[ok], is_output=True)

        held = {}

        def evac_q(ci, c0, M, tc0, n, ps, pkey):
            hd, part = divmod(ci, 2)
            if part == 0:
                held[(hd, tc0)] = (ps, pkey)
                return
            pn_, pnk_ = held.pop((hd, tc0))
            sq, sqk = cx.rm_sq[0], "rmsq0"
            sq2, sq2k = cx.rm_sq[1], "rmsq1"
            tk.op("act", lambda e: e.activation(out=sq[:, 0:n], in_=pn_[:, 0:n], func=AF.Square), r=[pnk_], w=[sqk])
            tk.op("act", lambda e: e.activation(out=sq2[0:64, 0:n], in_=ps[0:64, 0:n], func=AF.Square), r=[pkey], w=[sq2k])
            pss, pssk = cx.next_ps()
            tk.op("pe", lambda e: e.matmul(pss[:, 0:n], cx.ones[:], sq[:, 0:n], start=True, stop=False), r=[sqk, "ones"], w=[pssk])
            tk.op("pe", lambda e: e.matmul(pss[:, 0:n], cx.ones[0:64, :], sq2[0:64, 0:n], start=False, stop=True), r=[sq2k, "ones"], w=[pssk])
            rs, rsk = cx.rm_rstd, "rmrstd"
            tk.op("act", lambda e: e.activation(out=rs[:, 0:n], in_=pss[:, 0:n], func=AF.Sqrt, bias=eps1[:, 0:1], scale=1.0 / 192),
                  r=[pssk, "eps1"], w=[rsk])
            tk.op("dve", lambda e: e.reciprocal(out=rs[:, 0:n], in_=rs[:, 0:n]), r=[rsk], w=[rsk])
            o_, ok = nxt_ob()
            tk.op("dve", lambda e: e.scalar_tensor_tensor(out=o_[:, 0:n], in0=pn_[:, 0:n], scalar=gqk[:, 0:1], in1=rs[:, 0:n],
                                                          op0=ALU.mult, op1=ALU.mult), r=[pnk_, "gqk", rsk], w=[ok])
            tk.dma("sp", QnT[hd, :, tc0:tc0 + n], o_[:, 0:n], r=[ok], is_output=True)
            rope64(ps[0:64, 0:n], [pkey], 1, rs, rsk, tc0, n, QrT[hd, :, tc0:tc0 + n])

        chunks = []
        for hd in range(16):
            chunks += [(192 * hd, 128), (192 * hd + 128, 64)]
        linear_fm(cx, w_q_up, bufB[:, 0:4, :], lambda kc: kB(kc), 4, chunks, T, evac_q)

        def evac_kv(ci, c0, M, tc0, n, ps, pkey):
            hd, part = divmod(ci, 2)
            if part == 1:
                o_, ok = nxt_ob()
                tk.op("act", lambda e: e.activation(out=o_[:, 0:n], in_=ps[:, 0:n], func=AF.Copy), r=[pkey], w=[ok])
                tk.dma("sp", VT[hd, :, tc0:tc0 + n], o_[:, 0:n], r=[ok], is_output=True)
                return
            sq, sqk = cx.rm_sq[0], "rmsq0"
            tk.op("act", lambda e: e.activation(out=sq[:, 0:n], in_=ps[:, 0:n], func=AF.Square), r=[pkey], w=[sqk])
            pss, pssk = cx.next_ps()
            tk.op("pe", lambda e: e.matmul(pss[:, 0:n], cx.ones[:], sq[:, 0:n], start=True, stop=True), r=[sqk, "ones"], w=[pssk])
            rs, rsk = cx.rm_rstd, "rmrstd"
            tk.op("dve", lambda e: e.tensor_tensor(out=rs[:, 0:n], in0=pss[:, 0:n], in1=cf[:, 9, tc0:tc0 + n], op=ALU.add),
                  r=[pssk, ck(9)], w=[rsk])
            tk.op("act", lambda e: e.activation(out=rs[:, 0:n], in_=rs[:, 0:n], func=AF.Sqrt, bias=eps1[:, 0:1], scale=1.0 / 192),
                  r=[rsk, "eps1"], w=[rsk])
            tk.op("dve", lambda e: e.reciprocal(out=rs[:, 0:n], in_=rs[:, 0:n]), r=[rsk], w=[rsk])
            o_, ok = nxt_ob()
            tk.op("dve", lambda e: e.scalar_tensor_tensor(out=o_[:, 0:n], in0=ps[:, 0:n], scalar=gqk[:, 2:3], in1=rs[:, 0:n],
                                                          op0=ALU.mult, op1=ALU.mult), r=[pkey, "gqk", rsk], w=[ok])
            tk.dma("sp", KnT[hd, :, tc0:tc0 + n], o_[:, 0:n], r=[ok], is_output=True)
            rope64(cf[0:64, 8, tc0:tc0 + n], [ck(8)], 3, rs, rsk, tc0, n, KrT[hd, :, tc0:tc0 + n])

        chunks = []
        for hd in range(16):
            chunks += [(256 * hd, 128), (256 * hd + 128, 128)]
        linear_fm(cx, w_kv_up, bufB[:, 4:8, :], lambda kc: kB(4 + kc), 4, chunks, T, evac_kv)
        tk.finish()
    return nc


def build_mb_mla():
    nc = new_nc()
    Qn = din(nc, "Qn", [2, 128, S], BF16)
    Qr = din(nc, "Qr", [2, 64, S], BF16)
    Kn = din(nc, "Kn", [2, 128, S], BF16)
    Kr = din(nc, "Kr", [2, 64, S], BF16)
    V = din(nc, "V", [2, S, 128], BF16)
    oT = dout(nc, "oT", [2, 128, S], BF16)
    es = ExitStack()
    with es:
        cx = Ctx(nc, es, n_wbuf=0, n_ps=8, n_psb=0)
        tk = cx.tk
        scale = float(192 ** -0.5)
        sb_ = {}
        for h in range(2):
            sb_[h] = (tk.sb([128, S], BF16), tk.sb([64, S], BF16), tk.sb([128, S], BF16), tk.sb([64, S], BF16), tk.sb([128, 64, 128], BF16))
            qn, qr, kn, kr, vs = sb_[h]
            for c in range(4):
                sl = slice(c * 2048, (c + 1) * 2048)
                tk.dma("sp", qn[:, sl], Qn[h, :, sl], w=["qn%d_%d" % (h, c)])
                tk.dma("sp", qr[:, sl], Qr[h, :, sl], w=["qr%d_%d" % (h, c)])
                tk.dma("sp", kn[:, sl], Kn[h, :, sl], w=["kn%d_%d" % (h, c)])
                tk.dma("sp", kr[:, sl], Kr[h, :, sl], w=["kr%d_%d" % (h, c)])
                tk.dma("sp", vs[:, c * 16:(c + 1) * 16, :], V[h, sl, :].rearrange("(n p) c -> p n c", p=128), w=["v%d_%d" % (h, c)])
        pt = [tk.sb([128, 512], BF16) for _ in range(4)]
        ob = [tk.sb([128, 512], BF16) for _ in range(2)]
        rd = [tk.sb([128, 512], F32) for _ in range(2)]
        pi = 0
        qi = 0
        for h in range(2):
            qn, qr, kn, kr, vs = sb_[h]
            for qc in range(S // 512):
                qs = slice(qc * 512, (qc + 1) * 512)
                c4 = qc // 4
                po, pok = cx.psum[4 + 2 * (qi % 2)], "psum%d" % (4 + 2 * (qi % 2))
                pd, pdk = cx.psum[5 + 2 * (qi % 2)], "psum%d" % (5 + 2 * (qi % 2))
                LA = 2
                pend = {}
                for step in range(64 + LA):
                    if step < 64:
                        kt = step
                        ks = slice(kt * 128, (kt + 1) * 128)
                        kc4 = kt // 16
                        ps_, psk = cx.psum[pi % 4], "psum%d" % (pi % 4)
                        p_, pk = pt[pi % 4], "pt%d" % (pi % 4)
                        pi += 1
                        tk.op("pe", lambda e, ps_=ps_, ks=ks: e.matmul(ps_[:, :], kn[:, ks], qn[:, qs], start=True, stop=False),
                              r=["kn%d_%d" % (h, kc4), "qn%d_%d" % (h, c4)], w=[psk])
                        tk.op("pe", lambda e, ps_=ps_, ks=ks: e.matmul(ps_[:, :], kr[:, ks], qr[:, qs], start=False, stop=True),
                              r=["kr%d_%d" % (h, kc4), "qr%d_%d" % (h, c4)], w=[psk])
                        tk.op("act", lambda e, ps_=ps_, p_=p_: e.activation(out=p_[:], in_=ps_[:], func=AF.Exp, scale=scale), r=[psk], w=[pk])
                        pend[kt] = (p_, pk, kc4)
                    if step >= LA:
                        kt = step - LA
                        p_, pk, kc4 = pend.pop(kt)
                        tk.op("pe", lambda e, p_=p_, kt=kt: e.matmul(po[:, :], vs[:, kt, :], p_[:], start=(kt == 0), stop=(kt == 63)),
                              r=[pk, "v%d_%d" % (h, kc4)], w=[pok])
                        tk.op("pe", lambda e, p_=p_, kt=kt: e.matmul(pd[:, :], cx.ones[:], p_[:], start=(kt == 0), stop=(kt == 63)), r=[pk, "ones"], w=[pdk])
                r_, rk = rd[qi % 2], "rd%d" % (qi % 2)
                o_, ok = ob[qi % 2], "ob%d" % (qi % 2)
                qi += 1
                tk.op("dve", lambda e: e.reciprocal(out=r_[:], in_=pd[:]), r=[pdk], w=[rk])
                tk.op("dve", lambda e: e.tensor_tensor(out=o_[:], in0=po[:], in1=r_[:], op=ALU.mult), r=[pok, rk], w=[ok])
                tk.dma("sp", oT[h, :, qs], o_[:], r=[ok], is_output=True)
        tk.finish()
    return nc


def run_mla_layer(xT_full, inp, mod, layer, positions):
    nc = get_nc("ma_mla", build_ma_mla)
    qn_ = inp["mla_q_norm"][0]
    kn_ = inp["mla_k_norm"][0]
    gqk = np.zeros((128, 4), np.float32)
    gqk[:, 0] = qn_[0:128]
    gqk[0:64, 1] = qn_[128:192]
    gqk[:, 2] = kn_[0:128]
    gqk[0:64, 3] = kn_[128:192]
    ga = np.concatenate([np.asarray(inp["mla_q_a_norm"][0]).reshape(4, 128).T, np.asarray(inp["mla_kv_a_norm"][0]).reshape(4, 128).T], axis=1)
    maps = []
    for r in range(NCORES):
        maps.append({
            "xT": np.ascontiguousarray(xT_full[:, r * T:(r + 1) * T]),
            "modA": mod48(mod[layer, 0]), "gA": fm16(inp["norm_g"][layer, 0]),
            "w_in": np.ascontiguousarray(inp["mla_w_in"][0]), "w_q_up": np.ascontiguousarray(inp["mla_w_q_up"][0]),
            "w_kv_up": np.ascontiguousarray(inp["mla_w_kv_up"][0]),
            "pos": np.ascontiguousarray(positions[:, r * T:(r + 1) * T]).astype(np.int32),
            "invf": inv_freq(32, 64), "rotm": rot_matrix(32),
            "ga": np.ascontiguousarray(ga.astype(np.float32)), "gqk": gqk,
        })
    res = run(nc, maps)
    cat = lambda k: np.concatenate([res[r][k] for r in range(NCORES)], axis=2)
    QnT, QrT, KnT, KrT, VT = cat("QnT"), cat("QrT"), cat("KnT"), cat("KrT"), cat("VT")
    nc = get_nc("mb_mla", build_mb_mla)
    maps = []
    for r in range(NCORES):
        hs = slice(2 * r, 2 * r + 2)
        maps.append({"Qn": np.ascontiguousarray(QnT[hs]), "Qr": np.ascontiguousarray(QrT[hs]),
                     "Kn": np.ascontiguousarray(KnT[hs]), "Kr": np.ascontiguousarray(KrT[hs]),
                     "V": np.ascontiguousarray(VT[hs].transpose(0, 2, 1))})
    res = run(nc, maps)
    oT = np.concatenate([res[r]["oT"].reshape(256, S) for r in range(NCORES)], axis=0)
    return run_mc(xT_full, oT, inp["mla_w_out"][0], inp, mod, layer, 16)


def kernel(**inp):
    inp = {k: np.asarray(v) for k, v in inp.items()}
    x = inp["x"][0]
    positions = inp["positions"].astype(np.int32)
    mod = run_ada(inp["c"], inp["ada_w"], inp["ada_b"])
    xT = np.ascontiguousarray(x.T)
    for layer in range(4):
        mixer = layer % 4
        if mixer == 0:
            x1T, h2T, lgT = run_m_conv(xT, inp, mod, layer)
        elif mixer == 1:
            x1T, h2T, lgT = run_m_pool(xT, inp, mod, layer)
        elif mixer == 2:
            x1T, h2T, lgT = run_dil_layer(xT, inp, mod, layer, positions)
        else:
            x1T, h2T, lgT = run_mla_layer(xT, inp, mod, layer, positions)
        Y, inv = run_moe(h2T, lgT, inp, layer)
        xT = run_comb(x1T, Y, inv, np.ascontiguousarray(mod[layer, 1][2 * D:]))
    return np.ascontiguousarray(xT.T)[None].astype(np.float32)
```

```python
import numpy as np
import ml_dtypes
from contextlib import ExitStack
import concourse.bass as bass
import concourse.mybir as mybir
from concourse.bass_utils import run_bass_kernel_spmd

F32 = mybir.dt.float32
BF16 = mybir.dt.bfloat16
I32 = mybir.dt.int32
AF = mybir.ActivationFunctionType
ALU = mybir.AluOpType
AX = mybir.AxisListType
NPBF = ml_dtypes.bfloat16

NCORES = 8
D = 2048
S = 8192
T = 1024
KC = 16
EPS = 1e-6


class TK:
    NDS = 24

    def __init__(self, nc, es):
        self.nc = nc
        self.es = es
        self.engs = {"pe": nc.tensor, "act": nc.scalar, "dve": nc.vector,
                     "pool": nc.gpsimd, "sp": nc.sync}
        self.esem = {k: es.enter_context(nc.semaphore("sem_" + k)) for k in ("pe", "act", "dve", "pool")}
        self.ecnt = {k: 0 for k in self.esem}
        self.dsem = [es.enter_context(nc.semaphore("dsem%d" % i)) for i in range(self.NDS)]
        self.dcnt = [0] * self.NDS
        self.di = 0
        self.seen = {k: {} for k in self.engs}
        self.lastw = {}
        self.accw = {}
        self.reads = {}
        self.out_evs = []
        self.nsb = 0

    def sb(self, shape, dtype, name=None):
        self.nsb += 1
        return self.es.enter_context(self.nc.sbuf_tensor(name or ("sb%d" % self.nsb), list(shape), dtype))

    def ps(self, shape, dtype, name=None):
        self.nsb += 1
        return self.es.enter_context(self.nc.psum_tensor(name or ("ps%d" % self.nsb), list(shape), dtype))

    def _wait(self, eng, evs):
        seen = self.seen[eng]
        best = {}
        for (sem, val) in evs:
            if val <= 0:
                continue
            key = id(sem)
            if seen.get(key, 0) >= val:
                continue
            if key not in best or best[key][1] < val:
                best[key] = (sem, val)
        for key, (sem, val) in best.items():
            self.engs[eng].wait_ge(sem, val)
            seen[key] = val

    def _deps(self, eng, r, w, wacc=()):
        deps = []
        for k in r:
            if k in self.lastw:
                deps.append(self.lastw[k])
            deps.extend(self.accw.get(k, {}).values())
        for k in w:
            if k in self.lastw:
                deps.append(self.lastw[k])
            deps.extend(self.accw.get(k, {}).values())
            deps.extend(self.reads.get(k, {}).values())
        for k in wacc:
            if k in self.lastw:
                deps.append(self.lastw[k])
            deps.extend(self.reads.get(k, {}).values())
        if eng == "pe":
            own = id(self.esem["pe"])
            deps = [d for d in deps if id(d[0]) != own]
        return deps

    def _record(self, ev, r, w, wacc=()):
        for k in wacc:
            d = self.accw.setdefault(k, {})
            key = id(ev[0])
            if key not in d or d[key][1] < ev[1]:
                d[key] = ev
        for k in r:
            d = self.reads.setdefault(k, {})
            key = id(ev[0])
            if key not in d or d[key][1] < ev[1]:
                d[key] = ev
        for k in w:
            self.lastw[k] = ev
            self.reads[k] = {}
            self.accw[k] = {}

    def op(self, eng, fn, r=(), w=()):
        self._wait(eng, self._deps(eng, r, w))
        ins = fn(self.engs[eng])
        self.ecnt[eng] += 1
        ins.then_inc(self.esem[eng], 1)
        ev = (self.esem[eng], self.ecnt[eng])
        self._record(ev, r, w)
        return ev

    def dma(self, q, out, in_, r=(), w=(), is_output=False, **kw):
        self._wait(q, self._deps(q, r, w))
        i = self.di % self.NDS
        self.di += 1
        if self.dcnt[i] > 0:
            self._wait(q, [(self.dsem[i], self.dcnt[i])])
        ins = self.engs[q].dma_start(out=out, in_=in_, **kw)
        self.dcnt[i] += 16
        ins.then_inc(self.dsem[i], 16)
        ev = (self.dsem[i], self.dcnt[i])
        self._record(ev, r, w)
        if is_output:
            self.out_evs.append(ev)
        return ev

    def idma(self, out, out_off, in_, in_off, r=(), w=(), wacc=(), is_output=False, **kw):
        q = "pool"
        self._wait(q, self._deps(q, r, w, wacc))
        i = self.di % self.NDS
        self.di += 1
        if self.dcnt[i] > 0:
            self._wait(q, [(self.dsem[i], self.dcnt[i])])
        ins = self.nc.gpsimd.indirect_dma_start(out=out, out_offset=out_off, in_=in_, in_offset=in_off, **kw)
        self.dcnt[i] += 16
        ins.then_inc(self.dsem[i], 16)
        ev = (self.dsem[i], self.dcnt[i])
        self._record(ev, r, w, wacc)
        if is_output:
            self.out_evs.append(ev)
        return ev

    def finish(self):
        evs = list(self.out_evs)
        for i in range(self.NDS):
            if self.dcnt[i] > 0:
                evs.append((self.dsem[i], self.dcnt[i]))
        for k in self.esem:
            if self.ecnt[k] > 0:
                evs.append((self.esem[k], self.ecnt[k]))
        self._wait("sp", evs)


def new_nc():
    return bass.Bass("TRN2", target_bir_lowering=False)


def din(nc, name, shape, dtype):
    return nc.dram_tensor(name, list(shape), dtype, kind="ExternalInput").ap()


def dout(nc, name, shape, dtype):
    return nc.dram_tensor(name, list(shape), dtype, kind="ExternalOutput").ap()


def run(nc, in_maps):
    res = run_bass_kernel_spmd(nc, in_maps, core_ids=list(range(NCORES)))
    return res.results


def build_ada():
    nc = new_nc()
    cT = din(nc, "cT", [128, KC], F32)
    w = din(nc, "w", [D, 3 * D], F32)
    b = din(nc, "b", [1, 3 * D], F32)
    o = dout(nc, "mod", [1, 3 * D], F32)
    es = ExitStack()
    with es:
        tk = TK(nc, es)
        cs = tk.sb([128, KC], F32)
        cond = tk.sb([128, KC], F32)
        bsb = tk.sb([1, 3 * D], F32)
        osb = tk.sb([1, 3 * D], F32)
        wb = [tk.sb([128, KC, 512], F32) for _ in range(2)]
        pss = [tk.ps([128, 512], F32) for _ in range(2)]
        tk.dma("sp", cs[:], cT, w=["cs"])
        tk.dma("sp", bsb[:], b, w=["b"])
        tk.op("act", lambda e: e.activation(out=cond[:], in_=cs[:], func=AF.Silu), r=["cs"], w=["cond"])
        wv = w.rearrange("(kc p) f -> p kc f", p=128)
        for n in range(12):
            wbuf = wb[n % 2]
            tk.dma("sp", wbuf[:], wv[:, :, n * 512:(n + 1) * 512], w=["wb%d" % (n % 2)])
            ps = pss[n % 2]
            for kc in range(KC):
                tk.op("pe", lambda e, kc=kc: e.matmul(ps[0:1, :], cond[:, kc:kc + 1], wbuf[:, kc, :],
                                                      start=(kc == 0), stop=(kc == KC - 1)),
                      r=["cond", "wb%d" % (n % 2)], w=["ps%d" % (n % 2)])
            tk.op("dve", lambda e: e.tensor_tensor(out=osb[0:1, n * 512:(n + 1) * 512], in0=ps[0:1, :],
                                                   in1=bsb[0:1, n * 512:(n + 1) * 512], op=ALU.add),
                  r=["ps%d" % (n % 2), "b"], w=["osb%d" % n])
        tk.dma("sp", o, osb[:], r=["osb%d" % n for n in range(12)], is_output=True)
        tk.finish()
    return nc


def fm16(v):
    return np.ascontiguousarray(np.asarray(v).reshape(KC, 128).T)


def run_ada(c, ada_w, ada_b):
    nc = build_ada()
    cT = fm16(c[0])
    maps = []
    for r in range(NCORES):
        l, s = divmod(r, 2)
        maps.append({"cT": cT, "w": np.ascontiguousarray(ada_w[l, s]), "b": np.ascontiguousarray(ada_b[l, s][None, :])})
    res = run(nc, maps)
    return np.stack([res[r]["mod"][0] for r in range(NCORES)]).reshape(4, 2, 3 * D)


class Ctx:
    def __init__(self, nc, es, n_wbuf=3, wcols=512, wkc=KC, n_ps=6, n_psb=2):
        self.nc = nc
        self.tk = TK(nc, es)
        tk = self.tk
        self.psum = [tk.ps([128, 512], F32, name="psum%d" % i) for i in range(n_ps)]
        self.psi = 0
        self.psb = [tk.ps([128, 1024], BF16, name="psumb%d" % i) for i in range(n_psb)]
        self.psbi = 0
        self.wb = [tk.sb([128, wkc, wcols], BF16, name="wblk%d" % i) for i in range(n_wbuf)]
        self.wbi = 0
        self.ones = tk.sb([128, 128], BF16, name="ones_bf")
        tk.op("dve", lambda e: e.memset(self.ones[:], 1.0), w=["ones"])
        self.uid = 0
        self.epsD = tk.sb([128, 1], F32, name="epsD")
        tk.op("dve", lambda e: e.memset(self.epsD[:], float(D * EPS)), w=["epsD"])
        self.rm_sq = [tk.sb([128, 512], BF16) for _ in range(2)]
        self.rm_tmp = [tk.sb([128, 512], F32) for _ in range(2)]
        self.rm_hf = [tk.sb([128, 512], F32) for _ in range(2)]
        self.rm_rstd = tk.sb([128, 512], F32)

    def next_ps(self):
        i = self.psi % len(self.psum)
        self.psi += 1
        return self.psum[i], "psum%d" % i

    def next_psb(self):
        i = self.psbi % len(self.psb)
        self.psbi += 1
        return self.psb[i], "psumb%d" % i

    def next_wb(self):
        i = self.wbi % len(self.wb)
        self.wbi += 1
        return self.wb[i], "wblk%d" % i

    def key(self, s):
        self.uid += 1
        return "%s_%d" % (s, self.uid)


def tchunks(TT, n=512):
    return [(a, min(n, TT - a)) for a in range(0, TT, n)]


def linear_fm(cx, W, in_tile, in_keys, nk, out_chunks, TT, evac, t0=0):
    tk = cx.tk
    Wv = W.rearrange("(kc p) f -> p kc f", p=128)
    wcols = cx.wb[0].shape[2]
    blocks = []
    for ci, (c0, M) in enumerate(out_chunks):
        if blocks and blocks[-1][0] + blocks[-1][1] == c0 and blocks[-1][1] + M <= wcols:
            blocks[-1][1] += M
            blocks[-1][2].append((ci, c0, M))
        else:
            blocks.append([c0, M, [(ci, c0, M)]])
    for (b0, bw, chunks) in blocks:
        wb, wkey = cx.next_wb()
        tk.dma("pool", wb[:, 0:nk, 0:bw], Wv[:, :, b0:b0 + bw], w=[wkey])
        for (ci, c0, M) in chunks:
            for (tc0, n) in tchunks(TT):
                ps, pkey = cx.next_ps()
                for kc in range(nk):
                    tk.op("pe", lambda e, kc=kc: e.matmul(ps[0:M, 0:n], wb[:, kc, c0 - b0:c0 - b0 + M],
                                                          in_tile[:, kc, t0 + tc0:t0 + tc0 + n],
                                                          start=(kc == 0), stop=(kc == nk - 1)),
                          r=[wkey, in_keys(kc)], w=[pkey])
                evac(ci, c0, M, tc0, n, ps, pkey)


def load_mod(cx, mod_ap, g_ap):
    tk = cx.tk
    k = cx.key("mod")
    m = tk.sb([128, 48], F32)
    g = tk.sb([128, 16], F32)
    gs = tk.sb([128, 16], F32)
    g1 = tk.sb([128, 16], F32)
    tk.dma("sp", m[:], mod_ap, w=[k + "m"])
    tk.dma("sp", g[:], g_ap, w=[k + "g"])
    tk.op("dve", lambda e: e.scalar_tensor_tensor(out=gs[:], in0=m[:, 16:32], scalar=1.0, in1=g[:],
                                                  op0=ALU.add, op1=ALU.mult), r=[k + "m", k + "g"], w=[k + "gs0"])
    tk.op("dve", lambda e: e.tensor_scalar(out=gs[:], in0=gs[:], scalar1=float(np.sqrt(D)), scalar2=None,
                                           op0=ALU.mult), r=[k + "gs0"], w=[k + "gs"])
    tk.op("dve", lambda e: e.tensor_scalar(out=g1[:], in0=m[:, 32:48], scalar1=1.0, scalar2=None,
                                           op0=ALU.add), r=[k + "m"], w=[k + "g1"])
    return {"shift": m[:, 0:16], "gs": gs, "g1": g1, "kshift": k + "m", "kgs": k + "gs", "kg1": k + "g1"}


def rms_mod(cx, xs, xkeys, TT, t0, mod, h_out, hkey, router=None):
    tk = cx.tk
    sq, tmp, hf, rstd = cx.rm_sq, cx.rm_tmp, cx.rm_hf, cx.rm_rstd
    base = "rm"
    for ti, (tc0, n) in enumerate(tchunks(TT)):
        ps, pkey = cx.next_ps()
        for kc in range(KC):
            s = sq[kc % 2]
            sk = base + "sq%d" % (kc % 2)
            tk.op("act", lambda e: e.activation(out=s[:, 0:n], in_=xs[:, kc, t0 + tc0:t0 + tc0 + n], func=AF.Square),
                  r=[xkeys(kc)], w=[sk])
            tk.op("pe", lambda e: e.matmul(ps[:, 0:n], cx.ones[:], s[:, 0:n], start=(kc == 0), stop=(kc == KC - 1)),
                  r=[sk, "ones"], w=[pkey])
        rk = base + "rstd"
        tk.op("act", lambda e: e.activation(out=rstd[:, 0:n], in_=ps[:, 0:n], func=AF.Sqrt, bias=cx.epsD[:, 0:1], scale=1.0),
              r=[pkey, "epsD"], w=[rk + "0"])
        tk.op("dve", lambda e: e.reciprocal(out=rstd[:, 0:n], in_=rstd[:, 0:n]), r=[rk + "0"], w=[rk])
        for kc in range(KC):
            tm = tmp[kc % 2]
            tkk = base + "tmp%d" % (kc % 2)
            tk.op("dve", lambda e: e.tensor_tensor(out=tm[:, 0:n], in0=xs[:, kc, t0 + tc0:t0 + tc0 + n], in1=rstd[:, 0:n],
                                                   op=ALU.mult), r=[xkeys(kc), rk], w=[tkk])
            tk.op("act", lambda e: e.activation(out=h_out[:, kc, tc0:tc0 + n], in_=tm[:, 0:n], func=AF.Identity,
                                                bias=mod["shift"][:, kc:kc + 1], scale=mod["gs"][:, kc:kc + 1]),
                  r=[tkk, mod["kshift"], mod["kgs"]], w=[hkey(kc)])
            if router:
                hh = hf[kc % 2]
                hk = base + "hf%d" % (kc % 2)
                tk.op("act", lambda e: e.activation(out=hh[:, 0:n], in_=tm[:, 0:n], func=AF.Identity,
                                                    bias=mod["shift"][:, kc:kc + 1], scale=mod["gs"][:, kc:kc + 1]),
                      r=[tkk, mod["kshift"], mod["kgs"]], w=[hk])
                rps, rpk = router["ps"][ti], router["pkeys"][ti]
                tk.op("pe", lambda e: e.matmul(rps[0:16, 0:n], router["w"][:, kc, :], hh[:, 0:n],
                                               start=(kc == 0), stop=(kc == KC - 1)),
                      r=[hk, router["key"]], w=[rpk])


def load_xT(cx, xT_ap, TT):
    tk = cx.tk
    xs = tk.sb([128, KC, TT], F32, name="xs")
    xv = xT_ap.rearrange("(kc p) t -> p kc t", p=128)
    for kc in range(KC):
        tk.dma("sp", xs[:, kc, :], xv[:, kc, :], w=["x%d" % kc])
    return xs


def epilogue(cx, xs, halo, y_in, y_keys, nk_out, w_out_ap, modA, modB_ap, gB_ap, router_ap, outs, h2, h2key):
    tk = cx.tk

    def evac(ci, c0, M, tc0, n, ps, pkey):
        f = c0 // 128
        tk.op("dve", lambda e: e.scalar_tensor_tensor(out=xs[:, f, halo + tc0:halo + tc0 + n], in0=ps[:, 0:n],
                                                      scalar=modA["g1"][:, f:f + 1], in1=xs[:, f, halo + tc0:halo + tc0 + n],
                                                      op0=ALU.mult, op1=ALU.add),
              r=[pkey, modA["kg1"]], w=["x%d" % f])

    linear_fm(cx, w_out_ap, y_in, y_keys, nk_out, [(f * 128, 128) for f in range(KC)], T, evac)
    x1v = outs["x1T"].rearrange("(kc p) t -> p kc t", p=128)
    for kc in range(KC):
        tk.dma("sp", x1v[:, kc, :], xs[:, kc, halo:halo + T], r=["x%d" % kc], is_output=True)
    modB = load_mod(cx, modB_ap, gB_ap)
    rw = tk.sb([128, KC, 16], F32, name="router_w")
    tk.dma("sp", rw[:], router_ap.rearrange("(kc p) e -> p kc e", p=128), w=["router_w"])
    rps = []
    rpk = []
    for _ in tchunks(T):
        p, k = cx.next_ps()
        rps.append(p)
        rpk.append(k)
    rms_mod(cx, xs, lambda kc: "x%d" % kc, T, halo, modB, h2, h2key,
            router={"w": rw, "key": "router_w", "ps": rps, "pkeys": rpk})
    h2v = outs["h2T"].rearrange("(kc p) t -> p kc t", p=128)
    for kc in range(KC):
        tk.dma("sp", h2v[:, kc, :], h2[:, kc, 0:T], r=[h2key(kc)], is_output=True)
    lg = tk.sb([16, T], F32, name="lg")
    for ti, (tc0, n) in enumerate(tchunks(T)):
        tk.op("dve", lambda e: e.tensor_copy(lg[0:16, tc0:tc0 + n], rps[ti][0:16, 0:n]), r=[rpk[ti]], w=["lg%d" % ti])
    tk.dma("sp", outs["lgT"], lg[:], r=["lg%d" % ti for ti in range(len(tchunks(T)))], is_output=True)


def m_common_io(nc, halo):
    TT = T + 2 * halo
    io = {
        "xT": din(nc, "xT", [D, TT], F32),
        "modA": din(nc, "modA", [128, 48], F32),
        "modB": din(nc, "modB", [128, 48], F32),
        "gA": din(nc, "gA", [128, 16], F32),
        "gB": din(nc, "gB", [128, 16], F32),
        "router": din(nc, "router", [D, 16], F32),
        "x1T": dout(nc, "x1T", [D, T], F32),
        "h2T": dout(nc, "h2T", [D, T], BF16),
        "lgT": dout(nc, "lgT", [16, T], F32),
    }
    return io


POOL_HALO = 8


def build_m_pool():
    nc = new_nc()
    halo = POOL_HALO
    TT = T + 2 * halo
    io = m_common_io(nc, halo)
    w_in = din(nc, "w_in", [D, D], F32)
    w_grp = din(nc, "w_grp", [4, 512, 512], F32)
    ch_scale = din(nc, "ch_scale", [128, 16], F32)
    w_out = din(nc, "w_out", [D, D], F32)
    vmask = din(nc, "vmask", [1, TT], F32)
    invcnt = din(nc, "invcnt", [1, 4 * 16], F32)
    es = ExitStack()
    with es:
        cx = Ctx(nc, es, n_wbuf=2)
        tk = cx.tk
        xs = load_xT(cx, io["xT"], TT)
        modA = load_mod(cx, io["modA"], io["gA"])
        csc = tk.sb([128, 16], F32)
        tk.dma("sp", csc[:], ch_scale, w=["csc"])
        vm = tk.sb([128, TT], F32)
        tk.dma("sp", vm[:], vmask.to_broadcast([128, TT]), w=["vm"])
        ic = tk.sb([128, 64], F32)
        tk.dma("sp", ic[:], invcnt.to_broadcast([128, 64]), w=["ic"])
        bufA = tk.sb([128, KC, TT], BF16, name="bufA")
        bufB = tk.sb([128, KC, T], BF16, name="bufB")
        kA = lambda kc: "A%d" % kc
        kB = lambda kc: "B%d" % kc
        hT = bufA
        rms_mod(cx, xs, lambda kc: "x%d" % kc, TT, 0, modA, hT, kA)
        mixT = bufB
        ub = [tk.sb([128, TT], F32) for _ in range(2)]
        s1 = tk.sb([128, TT], F32)
        s2 = tk.sb([128, TT], F32)
        e8 = tk.sb([128, 16], F32)

        def evac_u(ci, c0, M, tc0, n, ps, pkey):
            f = ci
            u = ub[f % 2]
            uk = "u%d" % (f % 2)
            tk.op("act", lambda e: e.activation(out=u[:, tc0:tc0 + n], in_=ps[:, 0:n], func=AF.Copy), r=[pkey], w=[uk + "_%d" % tc0])
            if tc0 + n == TT:
                ukeys = [uk + "_%d" % a for (a, _) in tchunks(TT)]
                g = f // 4
                r = (1, 2, 4, 8)[g]
                tk.op("dve", lambda e: e.tensor_tensor(out=u[:, 0:halo], in0=u[:, 0:halo], in1=vm[:, 0:halo], op=ALU.mult),
                      r=ukeys + ["vm"], w=[uk + "_0"])
                tk.op("dve", lambda e: e.tensor_tensor(out=u[:, TT - halo:TT], in0=u[:, TT - halo:TT], in1=vm[:, TT - halo:TT], op=ALU.mult),
                      r=ukeys + ["vm"], w=[ukeys[-1]])
                src, srck, wdt = u, ukeys, 1
                bufs = [(s1, "s1"), (s2, "s2")]
                bi = 0
                while wdt < 2 * r:
                    dst, dk = bufs[bi % 2]
                    bi += 1
                    L = TT - 2 * wdt + 1
                    tk.op("dve", lambda e, src=src, dst=dst, wdt=wdt, L=L: e.tensor_tensor(
                        out=dst[:, 0:L], in0=src[:, 0:L], in1=src[:, wdt:wdt + L], op=ALU.add), r=srck, w=[dk])
                    src, srck, wdt = dst, [dk], 2 * wdt
                dst, dk = bufs[bi % 2]
                tk.op("dve", lambda e: e.tensor_tensor(out=dst[:, 0:T], in0=src[:, halo - r:halo - r + T],
                                                       in1=u[:, halo + r:halo + r + T], op=ALU.add), r=srck + ukeys, w=[dk])
                tk.op("dve", lambda e: e.scalar_tensor_tensor(out=mixT[:, f, :], in0=dst[:, 0:T], scalar=1.0 / (2 * r + 1),
                                                              in1=u[:, halo:halo + T], op0=ALU.mult, op1=ALU.subtract),
                      r=[dk] + ukeys, w=[kB(f)])
                for (a, ia) in ((0, 0), (T - 8, 8)):
                    tk.op("dve", lambda e, a=a, ia=ia: e.tensor_tensor(out=e8[:, ia:ia + 8], in0=dst[:, a:a + 8],
                                                                       in1=ic[:, g * 16 + ia:g * 16 + ia + 8], op=ALU.mult),
                          r=[dk, "ic"], w=["e8_%d" % ia])
                    tk.op("dve", lambda e, a=a, ia=ia: e.tensor_tensor(out=mixT[:, f, a:a + 8], in0=e8[:, ia:ia + 8],
                                                                       in1=u[:, halo + a:halo + a + 8], op=ALU.subtract),
                          r=["e8_%d" % ia] + ukeys, w=[kB(f)])

        linear_fm(cx, w_in, hT, kA, KC, [(f * 128, 128) for f in range(KC)], TT, evac_u)
        m2T = bufA
        for g in range(4):
            def evac_g(ci, c0, M, tc0, n, ps, pkey, g=g):
                f = 4 * g + ci
                tk.op("act", lambda e: e.activation(out=m2T[:, f, tc0:tc0 + n], in_=ps[:, 0:n], func=AF.Copy,
                                                    scale=csc[:, f:f + 1]), r=[pkey, "csc"], w=[kA(f)])
            linear_fm(cx, w_grp[g], mixT[:, 4 * g:4 * g + 4, :], lambda kc, g=g: kB(4 * g + kc), 4,
                      [(j * 128, 128) for j in range(4)], T, evac_g)
        epilogue(cx, xs, halo, m2T, kA, KC, w_out, modA, io["modB"], io["gB"], io["router"], io, bufB, kB)
        tk.finish()
    return nc


def mod48(m):
    return np.ascontiguousarray(np.concatenate([fm16(m[0:D]), fm16(m[D:2 * D]), fm16(m[2 * D:3 * D])], axis=1))


def shard_T(xT_full, r, halo):
    lo, hi = r * T - halo, (r + 1) * T + halo
    out = np.zeros((xT_full.shape[0], T + 2 * halo), xT_full.dtype)
    a, b = max(lo, 0), min(hi, S)
    out[:, a - lo:b - lo] = xT_full[:, a:b]
    return out


def common_maps(xT_full, inp, mod, layer, halo):
    maps = []
    for r in range(NCORES):
        maps.append({
            "xT": shard_T(xT_full, r, halo),
            "modA": mod48(mod[layer, 0]), "modB": mod48(mod[layer, 1]),
            "gA": fm16(inp["norm_g"][layer, 0]), "gB": fm16(inp["norm_g"][layer, 1]),
            "router": np.ascontiguousarray(inp["moe_router"][layer]),
        })
    return maps


def gather_common(res):
    x1T = np.concatenate([res[r]["x1T"] for r in range(NCORES)], axis=1)
    h2T = np.concatenate([res[r]["h2T"] for r in range(NCORES)], axis=1)
    lgT = np.concatenate([res[r]["lgT"] for r in range(NCORES)], axis=1)
    return x1T, h2T, lgT


_NC_CACHE = {}


def get_nc(name, builder):
    if name not in _NC_CACHE:
        _NC_CACHE[name] = builder()
    return _NC_CACHE[name]


def run_m_pool(xT_full, inp, mod, layer):
    nc = get_nc("m_pool", build_m_pool)
    halo = POOL_HALO
    maps = common_maps(xT_full, inp, mod, layer, halo)
    pos = np.arange(S)
    for r in range(NCORES):
        lo = r * T - halo
        tt = np.arange(lo, lo + T + 2 * halo)
        vmask = ((tt >= 0) & (tt < S)).astype(np.float32)[None, :]
        ic = np.zeros((4, 16), np.float32)
        for g, rr in enumerate((1, 2, 4, 8)):
            for ia, a in ((0, 0), (8, T - 8)):
                t = r * T + a + np.arange(8)
                cnt = np.minimum(t + rr + 1, S) - np.maximum(t - rr, 0)
                ic[g, ia:ia + 8] = 1.0 / cnt.astype(np.float32)
        maps[r].update({
            "w_in": np.ascontiguousarray(inp["pool_w_in"][0]), "w_grp": np.ascontiguousarray(inp["pool_w_grp"][0]),
            "ch_scale": fm16(inp["pool_scale"][0]), "w_out": np.ascontiguousarray(inp["pool_w_out"][0]),
            "vmask": vmask, "invcnt": ic.reshape(1, 64),
        })
    return gather_common(run(nc, maps))


CAP = 1024
BIGSLOT = 16384.0
BIS_ITERS = 30


def build_moe():
    nc = new_nc()
    lg = din(nc, "lg", [S, 16], F32)
    h2 = din(nc, "h2", [S, D], BF16)
    wg = din(nc, "wg", [2, D, 1024], F32)
    wu = din(nc, "wu", [2, D, 1024], F32)
    wd = din(nc, "wd", [2, 1024, D], F32)
    ltri = din(nc, "ltri", [128, 128], F32)
    ident = din(nc, "ident", [128, 128], F32)
    Y = dout(nc, "Y", [2, CAP, D], BF16)
    inv = dout(nc, "inv", [2, S], I32)
    idxl = [nc.dram_tensor("idxl%d" % i, [CAP + 8, 2], I32).ap() for i in range(2)]
    es = ExitStack()
    with es:
        cx = Ctx(nc, es, n_wbuf=4)
        tk = cx.tk
        lgs = tk.sb([128, 64, 16], F32)
        tk.dma("sp", lgs[:], lg.rearrange("(p j) e -> p j e", j=64), w=["lgs"])
        lt = tk.sb([128, 128], F32)
        tk.dma("sp", lt[:], ltri, w=["lt"])
        idb = tk.sb([128, 128], BF16)
        tk.dma("pool", idb[:], ident, w=["idb"])
        wdsb = tk.sb([128, 8, D], BF16, name="wdsb")

        def load_gu(e_):
            wgv = wg[e_].rearrange("(kc p) f -> p kc f", p=128)
            wuv = wu[e_].rearrange("(kc p) f -> p kc f", p=128)
            out = []
            for fb in range(2):
                wgb, wgk = cx.next_wb()
                tk.dma("pool", wgb[:], wgv[:, :, fb * 512:(fb + 1) * 512], w=[wgk])
                wub, wuk = cx.next_wb()
                tk.dma("pool", wub[:], wuv[:, :, fb * 512:(fb + 1) * 512], w=[wuk])
                out.append((wgb, wgk, wub, wuk))
            return out

        def load_wd(e_):
            wdv = wd[e_].rearrange("(kc p) f -> p kc f", p=128)
            for db in range(4):
                tk.dma("pool", wdsb[:, :, db * 512:(db + 1) * 512], wdv[:, :, db * 512:(db + 1) * 512], w=["wd%d" % db])

        gu = {0: load_gu(0)}
        load_wd(0)
        onesf = tk.sb([128, 128], F32)
        tk.op("dve", lambda e: e.memset(onesf[:], 1.0), w=["onesf"])
        ones64 = tk.sb([128, 64], F32)
        tk.op("dve", lambda e: e.memset(ones64[:], 1.0), w=["ones64"])
        mx = tk.sb([128, 64], F32)
        tk.op("dve", lambda e: e.tensor_reduce(out=mx[:], in_=lgs[:], axis=AX.X, op=ALU.max), r=["lgs"], w=["mx"])
        ex = tk.sb([128, 64, 16], F32)
        tk.op("dve", lambda e: e.tensor_tensor(out=ex[:], in0=lgs[:], in1=mx[:].unsqueeze(2).to_broadcast([128, 64, 16]),
                                               op=ALU.subtract), r=["lgs", "mx"], w=["ex0"])
        tk.op("act", lambda e: e.activation(out=ex[:], in_=ex[:], func=AF.Exp), r=["ex0"], w=["ex"])
        sm = tk.sb([128, 64], F32)
        tk.op("dve", lambda e: e.tensor_reduce(out=sm[:], in_=ex[:], axis=AX.X, op=ALU.add), r=["ex"], w=["sm0"])
        tk.op("dve", lambda e: e.reciprocal(out=sm[:], in_=sm[:]), r=["sm0"], w=["sm"])
        aff = tk.sb([128, 2, 64], F32)
        for e_ in range(2):
            tk.op("dve", lambda e, e_=e_: e.tensor_tensor(out=aff[:, e_, :], in0=ex[:, :, e_], in1=sm[:], op=ALU.mult),
                  r=["ex", "sm"], w=["aff%d" % e_])
        affk = ["aff0", "aff1"]
        lo = tk.sb([128, 2], F32)
        cand = tk.sb([128, 2], F32)
        cmp_ = tk.sb([128, 2, 64], F32)
        cnt = tk.sb([128, 2], F32)
        ge = tk.sb([128, 2], F32)
        tk.op("dve", lambda e: e.memset(lo[:], 0.0), w=["lo"])
        ps_b, pk_b = cx.next_ps()
        for it in range(BIS_ITERS):
            step = float(2.0 ** -(it + 1))
            tk.op("dve", lambda e: e.tensor_scalar(out=cand[:], in0=lo[:], scalar1=step, scalar2=None, op0=ALU.add),
                  r=["lo"], w=["cand"])
            tk.op("dve", lambda e: e.tensor_tensor(out=cmp_[:], in0=aff[:], in1=cand[:].unsqueeze(2).to_broadcast([128, 2, 64]),
                                                   op=ALU.is_ge), r=affk + ["cand"], w=["cmp"])
            tk.op("dve", lambda e: e.tensor_reduce(out=cnt[:], in_=cmp_[:], axis=AX.X, op=ALU.add), r=["cmp"], w=["cnt"])
            tk.op("pe", lambda e: e.matmul(ps_b[:, 0:2], onesf[:], cnt[:], start=True, stop=True), r=["cnt", "onesf"], w=[pk_b])
            tk.op("dve", lambda e: e.tensor_scalar(out=ge[:], in0=ps_b[:, 0:2], scalar1=float(CAP), scalar2=None, op0=ALU.is_ge),
                  r=[pk_b], w=["ge"])
            tk.op("dve", lambda e: e.scalar_tensor_tensor(out=lo[:], in0=ge[:], scalar=step, in1=lo[:], op0=ALU.mult, op1=ALU.add),
                  r=["ge", "lo"], w=["lo"])
        mask = tk.sb([128, 2, 64], F32)
        tk.op("dve", lambda e: e.tensor_tensor(out=mask[:], in0=aff[:], in1=lo[:].unsqueeze(2).to_broadcast([128, 2, 64]),
                                               op=ALU.is_ge), r=affk + ["lo"], w=["mask"])
        tk.op("dve", lambda e: e.tensor_reduce(out=cnt[:], in_=mask[:], axis=AX.X, op=ALU.add), r=["mask"], w=["cnt"])
        tk.op("pe", lambda e: e.matmul(ps_b[:, 0:2], lt[:], cnt[:], start=True, stop=True), r=["cnt", "lt"], w=[pk_b])
        offs = tk.sb([128, 2], F32)
        tk.op("dve", lambda e: e.tensor_copy(offs[:], ps_b[:, 0:2]), r=[pk_b], w=["offs"])
        cs = tk.sb([128, 2, 64], F32)
        pos = tk.sb([128, 2, 64], F32)
        seli = tk.sb([128, 2, 64], I32)
        tok = tk.sb([128, 64], I32)
        tk.op("pool", lambda e: e.iota(tok[:], pattern=[[1, 64]], base=0, channel_multiplier=64), w=["tok"])
        pay = tk.sb([128, 64, 2, 2], I32)
        payf = pay[:].bitcast(F32)
        invv = inv.rearrange("e (p j) -> e p j", j=64)
        for e_ in range(2):
            tk.op("dve", lambda e, e_=e_: e.tensor_tensor_scan(out=cs[:, e_, :], data0=ones64[:], data1=mask[:, e_, :], initial=0.0,
                                                               op0=ALU.mult, op1=ALU.add), r=["mask", "ones64"], w=["cs%d" % e_])
            tk.op("dve", lambda e, e_=e_: e.scalar_tensor_tensor(out=pos[:, e_, :], in0=cs[:, e_, :], scalar=offs[:, e_:e_ + 1],
                                                                 in1=mask[:, e_, :], op0=ALU.add, op1=ALU.subtract),
                  r=["cs%d" % e_, "offs", "mask"], w=["pos%d" % e_])
            tk.op("dve", lambda e, e_=e_: e.scalar_tensor_tensor(out=pos[:, e_, :], in0=pos[:, e_, :], scalar=-BIGSLOT,
                                                                 in1=mask[:, e_, :], op0=ALU.add, op1=ALU.mult),
                  r=["pos%d" % e_, "mask"], w=["pos%d" % e_])
            tk.op("dve", lambda e, e_=e_: e.tensor_scalar(out=seli[:, e_, :], in0=pos[:, e_, :], scalar1=BIGSLOT, scalar2=None,
                                                          op0=ALU.add), r=["pos%d" % e_], w=["seli%d" % e_])
            tk.dma("sp", invv[e_], seli[:, e_, :], r=["seli%d" % e_], is_output=True)
            tk.op("dve", lambda e, e_=e_: e.tensor_copy(pay[:, :, e_, 0], tok[:]), r=["tok"], w=["pay%d" % e_])
            tk.op("dve", lambda e, e_=e_: e.tensor_copy(payf[:, :, e_, 1], aff[:, e_, :]), r=[affk[e_], "pay%d" % e_], w=["pay%d" % e_])
        breg = nc.gpsimd.to_reg(CAP - 1)
        idxsb = tk.sb([128, 2, 8, 2], I32)
        idxf = idxsb[:].bitcast(F32)

        def scatter_lists(e_):
            for j in range(64):
                tk.idma(out=idxl[e_], out_off=bass.IndirectOffsetOnAxis(ap=seli[:, e_, j:j + 1], axis=0),
                        in_=pay[:, j, e_, :], in_off=None, r=["seli%d" % e_, "pay%d" % e_], wacc=["idxl%d" % e_],
                        bounds_check=breg, oob_is_err=False)
            tk.dma("sp", idxsb[:, e_, :, :], idxl[e_][0:CAP, :].rearrange("(i p) w -> p i w", p=128), r=["idxl%d" % e_], w=["idxsb%d" % e_])
        xsT = tk.sb([128, KC, CAP], BF16, name="xsT")
        actT = tk.sb([128, 8, CAP], BF16, name="actT")
        xg = [tk.sb([128, D], BF16) for _ in range(2)]
        sa = [tk.sb([128, 512], F32) for _ in range(2)]
        ysb = [tk.sb([128, D], BF16) for _ in range(2)]
        gi = 0
        si = 0
        yi = 0

        def gather_rows(e_):
            nonlocal gi
            for i in range(8):
                g = xg[gi % 2]
                gk = "xg%d" % (gi % 2)
                gi += 1
                tk.idma(out=g[:], out_off=None, in_=h2, in_off=bass.IndirectOffsetOnAxis(ap=idxsb[:, e_, i, 0:1], axis=0),
                        r=["idxsb%d" % e_], w=[gk])
                for half in range(2):
                    pb, pbk = cx.next_psb()
                    for q in range(8):
                        kc = half * 8 + q
                        tk.op("pe", lambda e, kc=kc, q=q: e.transpose(pb[:, q * 128:(q + 1) * 128], g[:, kc * 128:(kc + 1) * 128], idb[:]),
                              r=[gk, "idb"], w=[pbk])
                    tk.op("act" if half == 0 else "dve",
                          lambda e, half=half: (e.activation(out=xsT[:, half * 8:half * 8 + 8, i * 128:(i + 1) * 128],
                                                             in_=pb[:].rearrange("p (q t) -> p q t", t=128), func=AF.Copy)
                                                if half == 0 else
                                                e.tensor_copy(xsT[:, half * 8:half * 8 + 8, i * 128:(i + 1) * 128],
                                                              pb[:].rearrange("p (q t) -> p q t", t=128))),
                          r=[pbk], w=["xsT_%d_%d" % (half, i)])

        scatter_lists(0)
        gather_rows(0)
        scatter_lists(1)
        for e_ in range(2):
            xkeys = ["xsT_%d_%d" % (h_, i) for h_ in range(2) for i in range(8)]
            for fb in range(2):
                wgb, wgk, wub, wuk = gu[e_][fb]
                for fc in range(4):
                    f = fb * 4 + fc
                    for th in range(2):
                        pa, pak = cx.next_ps()
                        pbb, pbbk = cx.next_ps()
                        for kc in range(KC):
                            tk.op("pe", lambda e, kc=kc: e.matmul(pa[:, :], wgb[:, kc, fc * 128:(fc + 1) * 128],
                                                                  xsT[:, kc, th * 512:(th + 1) * 512], start=(kc == 0), stop=(kc == KC - 1)),
                                  r=[wgk] + xkeys, w=[pak])
                        for kc in range(KC):
                            tk.op("pe", lambda e, kc=kc: e.matmul(pbb[:, :], wub[:, kc, fc * 128:(fc + 1) * 128],
                                                                  xsT[:, kc, th * 512:(th + 1) * 512], start=(kc == 0), stop=(kc == KC - 1)),
                                  r=[wuk] + xkeys, w=[pbbk])
                        s_ = sa[si % 2]
                        sk = "sa%d" % (si % 2)
                        si += 1
                        tk.op("act", lambda e: e.activation(out=s_[:], in_=pa[:], func=AF.Silu), r=[pak], w=[sk])
                        tk.op("dve", lambda e: e.tensor_tensor(out=actT[:, f, th * 512:(th + 1) * 512], in0=s_[:], in1=pbb[:], op=ALU.mult),
                              r=[sk, pbbk], w=["actT%d_%d" % (f, th)])
            akeys = ["actT%d_%d" % (f, th) for f in range(8) for th in range(2)]
            if e_ == 0:
                gu[1] = load_gu(1)
                gather_rows(1)
            for i in range(8):
                y_ = ysb[yi % 2]
                yk = "ysb%d" % (yi % 2)
                yi += 1
                for db in range(4):
                    po, pok = cx.next_ps()
                    for f in range(8):
                        tk.op("pe", lambda e, f=f: e.matmul(po[:, :], actT[:, f, i * 128:(i + 1) * 128], wdsb[:, f, db * 512:(db + 1) * 512],
                                                            start=(f == 0), stop=(f == 7)), r=akeys + ["wd%d" % db], w=[pok])
                    tk.op("act" if db % 2 == 0 else "dve",
                          lambda e, db=db: (e.activation(out=y_[:, db * 512:(db + 1) * 512], in_=po[:], func=AF.Copy,
                                                         scale=idxf[:, e_, i, 1:2]) if db % 2 == 0 else
                                            e.tensor_scalar(out=y_[:, db * 512:(db + 1) * 512], in0=po[:], scalar1=idxf[:, e_, i, 1:2],
                                                            scalar2=None, op0=ALU.mult)),
                          r=[pok, "idxsb%d" % e_], w=[yk + "_%d" % db])
                tk.dma("sp", Y[e_, i * 128:(i + 1) * 128, :], y_[:], r=[yk + "_%d" % db for db in range(4)], is_output=True)
            if e_ == 0:
                load_wd(1)
        tk.finish()
    return nc


def moe_consts():
    lt = (np.arange(128)[:, None] < np.arange(128)[None, :]).astype(np.float32)
    return {"ltri": lt, "ident": np.eye(128, dtype=np.float32)}


def run_moe(h2T_full, lgT_full, inp, layer):
    nc = get_nc("moe", build_moe)
    h2 = np.ascontiguousarray(h2T_full.T)
    lg = np.ascontiguousarray(lgT_full.T)
    cst = moe_consts()
    maps = []
    for r in range(NCORES):
        perm = [2 * r, 2 * r + 1] + [e for e in range(16) if e not in (2 * r, 2 * r + 1)]
        m = {"lg": np.ascontiguousarray(lg[:, perm]), "h2": h2,
             "wg": np.ascontiguousarray(inp["moe_w_gate"][layer, 2 * r:2 * r + 2]),
             "wu": np.ascontiguousarray(inp["moe_w_up"][layer, 2 * r:2 * r + 2]),
             "wd": np.ascontiguousarray(inp["moe_w_down"][layer, 2 * r:2 * r + 2])}
        m.update(cst)
        maps.append(m)
    res = run(nc, maps)
    Y = np.concatenate([res[r]["Y"] for r in range(NCORES)], axis=0)
    inv = np.concatenate([res[r]["inv"] for r in range(NCORES)], axis=0)
    return Y, inv


def build_comb():
    nc = new_nc()
    x1 = din(nc, "x1", [T, D], F32)
    Ye = [din(nc, "Y%d" % e, [CAP, D], BF16) for e in range(16)]
    invp = din(nc, "invp", [128, 16 * 8], I32)
    gate = din(nc, "gate", [1, D], F32)
    ident = din(nc, "ident", [128, 128], F32)
    xo = dout(nc, "xo", [T, D], F32)
    es = ExitStack()
    with es:
        tk = TK(nc, es)
        psum = [tk.ps([128, 512], F32, name="psum%d" % i) for i in range(8)]
        invsb = tk.sb([128, 128], I32)
        tk.dma("sp", invsb[:], invp, w=["inv"])
        idb = tk.sb([128, 128], BF16)
        tk.dma("pool", idb[:], ident, w=["idb"])
        g1b = tk.sb([128, D], F32)
        tk.dma("sp", g1b[:], gate.to_broadcast([128, D]), w=["g1b0"])
        tk.op("dve", lambda e: e.tensor_scalar(out=g1b[:], in0=g1b[:], scalar1=1.0, scalar2=None, op0=ALU.add), r=["g1b0"], w=["g1b"])
        NZ = 8
        Z = [tk.sb([128, D], BF16) for _ in range(NZ)]
        zt = tk.sb([128, D], BF16)
        tk.op("dve", lambda e: e.memset(zt[:], 0.0), w=["zt"])
        acc = [tk.sb([128, D], F32) for _ in range(2)]
        xt = [tk.sb([128, D], F32) for _ in range(2)]
        breg = nc.gpsimd.to_reg(CAP - 1)
        zi = 0
        for i in range(8):
            a = acc[i % 2]
            ak = "acc%d" % (i % 2)
            x_ = xt[i % 2]
            xk = "xt%d" % (i % 2)
            tk.dma("sp", x_[:], x1[i * 128:(i + 1) * 128, :], w=[xk])
            pbank = [(psum[(i % 2) * 4 + c], "psum%d" % ((i % 2) * 4 + c)) for c in range(4)]
            for e_ in range(16):
                z = Z[zi % NZ]
                zk = "Z%d" % (zi % NZ)
                zi += 1
                if zi % 2 == 0:
                    tk.op("dve", lambda e, z=z: e.memset(z[:], 0.0), w=[zk])
                else:
                    tk.op("act", lambda e, z=z: e.activation(out=z[:], in_=zt[:], func=AF.Copy), r=["zt"], w=[zk])
                tk.idma(out=z[:], out_off=None, in_=Ye[e_], in_off=bass.IndirectOffsetOnAxis(ap=invsb[:, e_ * 8 + i:e_ * 8 + i + 1], axis=0),
                        r=["inv"], w=[zk], bounds_check=breg, oob_is_err=False)
                for c in range(4):
                    ps, pk = pbank[c]
                    tk.op("pe", lambda e, z=z, ps=ps, c=c, e_=e_: e.matmul(ps[:, :], idb[:], z[:, c * 512:(c + 1) * 512],
                                                                        start=(e_ == 0), stop=(e_ == 15)), r=[zk, "idb"], w=[pk])
            for c in range(4):
                ps, pk = pbank[c]
                cs = slice(c * 512, (c + 1) * 512)
                tk.op("dve", lambda e, ps=ps, cs=cs: e.tensor_tensor(out=a[:, cs], in0=ps[:, :], in1=g1b[:, cs], op=ALU.mult), r=[pk, "g1b"], w=[ak + "_%d" % c])
                tk.op("dve", lambda e, cs=cs: e.tensor_tensor(out=a[:, cs], in0=a[:, cs], in1=x_[:, cs], op=ALU.add), r=[ak + "_%d" % c, xk], w=[ak + "_%d" % c])
            tk.dma("sp", xo[i * 128:(i + 1) * 128, :], a[:], r=[ak + "_%d" % c for c in range(4)], is_output=True)
        tk.finish()
    return nc


def run_comb(x1T_full, Y, inv, gate_vec):
    nc = get_nc("comb", build_comb)
    maps = []
    for r in range(NCORES):
        iv = inv[:, r * T:(r + 1) * T].reshape(16, 8, 128)
        invp = np.ascontiguousarray(iv.transpose(2, 0, 1).reshape(128, 128))
        m = {"x1": np.ascontiguousarray(x1T_full[:, r * T:(r + 1) * T].T), "invp": invp,
             "gate": np.ascontiguousarray(gate_vec[None, :]), "ident": np.eye(128, dtype=np.float32)}
        for e in range(16):
            m["Y%d" % e] = Y[e]
        maps.append(m)
    res = run(nc, maps)
    return np.ascontiguousarray(np.concatenate([res[r]["xo"] for r in range(NCORES)], axis=0).T)


CONV_HALO = 15


def build_m_conv():
    nc = new_nc()
    halo = CONV_HALO
    TT = T + 2 * halo
    io = m_common_io(nc, halo)
    w_in = din(nc, "w_in", [D, 2 * D], F32)
    dwp = din(nc, "dw", [128, KC * 31], F32)
    lng = din(nc, "lng", [128, 16], F32)
    lnb = din(nc, "lnb", [128, 16], F32)
    w_out = din(nc, "w_out", [D, D], F32)
    vme = din(nc, "vme", [1, 2 * KC * halo], F32)
    ident_ap = din(nc, "ident", [128, 128], F32)
    es = ExitStack()
    with es:
        cx = Ctx(nc, es, n_wbuf=2)
        tk = cx.tk
        xs = load_xT(cx, io["xT"], TT)
        xk = lambda kc: "x%d" % kc
        modA = load_mod(cx, io["modA"], io["gA"])
        dws = tk.sb([128, KC, 31], F32)
        tk.dma("sp", dws[:], dwp.rearrange("p (kc k) -> p kc k", k=31), w=["dw"])
        identf = tk.sb([128, 128], F32)
        tk.dma("sp", identf[:], ident_ap, w=["identf"])
        lg_ = tk.sb([128, 16], F32)
        lb_ = tk.sb([128, 16], F32)
        tk.dma("sp", lg_[:], lng, w=["lng"])
        tk.dma("sp", lb_[:], lnb, w=["lnb"])
        vm = tk.sb([128, 2, KC, halo], F32)
        tk.dma("sp", vm[:], vme.rearrange("o (s kc h) -> o s kc h", s=2, kc=KC).to_broadcast([128, 2, KC, halo]), w=["vm"])
        eps1 = tk.sb([128, 1], F32)
        tk.op("dve", lambda e: e.memset(eps1[:], float(EPS)), w=["eps1"])
        bufA = tk.sb([128, KC, TT], BF16, name="bufA")
        bufB = tk.sb([128, KC, TT], BF16, name="bufB")
        kA = lambda kc: "A%d" % kc
        kB = lambda kc: "B%d" % kc
        rms_mod(cx, xs, xk, TT, 0, modA, bufA, kA)
        a_tmp = tk.sb([128, 4, TT], BF16)
        sg = cx.rm_tmp
        sgi = [0]
        for blk in range(4):
            def evac_a(ci, c0, M, tc0, n, ps, pkey, blk=blk):
                tk.op("act", lambda e: e.activation(out=a_tmp[:, ci, tc0:tc0 + n], in_=ps[:, 0:n], func=AF.Copy),
                      r=[pkey], w=["atmp%d_%d" % (ci, tc0)])

            def evac_b(ci, c0, M, tc0, n, ps, pkey, blk=blk):
                f = blk * 4 + ci
                s_ = sg[sgi[0] % 2]
                sk = "rmtmp%d" % (sgi[0] % 2)
                sgi[0] += 1
                tk.op("act", lambda e: e.activation(out=s_[:, 0:n], in_=ps[:, 0:n], func=AF.Sigmoid), r=[pkey], w=[sk])
                tk.op("dve", lambda e: e.tensor_tensor(out=bufB[:, f, tc0:tc0 + n], in0=s_[:, 0:n], in1=a_tmp[:, ci, tc0:tc0 + n], op=ALU.mult),
                      r=[sk, "atmp%d_%d" % (ci, tc0)], w=[kB(f)])
            linear_fm(cx, w_in, bufA, kA, KC, [((blk * 4 + j) * 128, 128) for j in range(4)], TT, evac_a)
            linear_fm(cx, w_in, bufA, kA, KC, [(D + (blk * 4 + j) * 128, 128) for j in range(4)], TT, evac_b)
        allB = [kB(f) for f in range(KC)]
        tk.op("dve", lambda e: e.tensor_tensor(out=bufB[:, :, 0:halo], in0=bufB[:, :, 0:halo], in1=vm[:, 0, :, :], op=ALU.mult),
              r=allB + ["vm"], w=allB)
        tk.op("dve", lambda e: e.tensor_tensor(out=bufB[:, :, TT - halo:TT], in0=bufB[:, :, TT - halo:TT], in1=vm[:, 1, :, :], op=ALU.mult),
              r=allB + ["vm"], w=allB)
        dgs = [cx.wb[i][:].rearrange("p a b -> p (a b)")[:, 0:31 * 128].rearrange("p (k m) -> p k m", m=128) for i in range(2)]
        for f in range(KC):
            dg, dgk = dgs[f % 2], "wblk%d" % (f % 2)
            for k in range(31):
                if k % 3 == 2:
                    tk.op("act", lambda e, k=k: e.activation(out=dg[:, k, :], in_=identf[:], func=AF.Copy, scale=dws[:, f, k:k + 1]),
                          r=["identf", "dw"], w=[dgk])
                else:
                    tk.op("dve", lambda e, k=k: e.tensor_scalar(out=dg[:, k, :], in0=identf[:], scalar1=dws[:, f, k:k + 1], scalar2=None, op0=ALU.mult),
                          r=["identf", "dw"], w=[dgk])
            for (tc0, n) in tchunks(T):
                ps, pkey = cx.next_ps()
                for k in range(31):
                    tk.op("pe", lambda e, k=k: e.matmul(ps[:, 0:n], dg[:, k, :], bufB[:, f, k + tc0:k + tc0 + n], start=(k == 0), stop=(k == 30)),
                          r=[dgk, kB(f)], w=[pkey])
                tk.op("act", lambda e: e.activation(out=xs[:, f, tc0:tc0 + n], in_=ps[:, 0:n], func=AF.Copy), r=[pkey], w=[xk(f)])
        mu = tk.sb([128, T], F32)
        rstd = tk.sb([128, T], F32)
        msq = cx.rm_hf[0]
        cb = [tk.sb([128, 512], BF16) for _ in range(2)]
        cq = [tk.sb([128, 512], BF16) for _ in range(2)]
        for ti, (tc0, n) in enumerate(tchunks(T)):
            p1, k1 = cx.next_ps()
            p2, k2 = cx.next_ps()
            for f in range(KC):
                b1, b1k = cb[f % 2], "cb%d" % (f % 2)
                b2, b2k = cq[f % 2], "cq%d" % (f % 2)
                tk.op("act", lambda e: e.activation(out=b1[:, 0:n], in_=xs[:, f, tc0:tc0 + n], func=AF.Copy), r=[xk(f)], w=[b1k])
                tk.op("act", lambda e: e.activation(out=b2[:, 0:n], in_=xs[:, f, tc0:tc0 + n], func=AF.Square), r=[xk(f)], w=[b2k])
                tk.op("pe", lambda e: e.matmul(p1[:, 0:n], cx.ones[:], b1[:, 0:n], start=(f == 0), stop=(f == KC - 1)), r=[b1k, "ones"], w=[k1])
                tk.op("pe", lambda e: e.matmul(p2[:, 0:n], cx.ones[:], b2[:, 0:n], start=(f == 0), stop=(f == KC - 1)), r=[b2k, "ones"], w=[k2])
            tk.op("dve", lambda e: e.tensor_scalar(out=mu[:, tc0:tc0 + n], in0=p1[:, 0:n], scalar1=1.0 / D, scalar2=None, op0=ALU.mult),
                  r=[k1], w=["mu%d" % ti])
            tk.op("dve", lambda e: e.tensor_tensor(out=msq[:, 0:n], in0=mu[:, tc0:tc0 + n], in1=mu[:, tc0:tc0 + n], op=ALU.mult),
                  r=["mu%d" % ti], w=["rmhf0"])
            tk.op("dve", lambda e: e.scalar_tensor_tensor(out=rstd[:, tc0:tc0 + n], in0=p2[:, 0:n], scalar=1.0 / D, in1=msq[:, 0:n],
                                                          op0=ALU.mult, op1=ALU.subtract), r=[k2, "rmhf0"], w=["rstd%d" % ti])
            tk.op("act", lambda e: e.activation(out=rstd[:, tc0:tc0 + n], in_=rstd[:, tc0:tc0 + n], func=AF.Sqrt, bias=eps1[:, 0:1], scale=1.0),
                  r=["rstd%d" % ti, "eps1"], w=["rstd%d" % ti])
            tk.op("dve", lambda e: e.reciprocal(out=rstd[:, tc0:tc0 + n], in_=rstd[:, tc0:tc0 + n]), r=["rstd%d" % ti], w=["rstd%d" % ti])
        stat_keys = ["mu0", "mu1", "rstd0", "rstd1"]
        for f in range(KC):
            c = xs[:, f, 0:T]
            tk.op("dve", lambda e: e.tensor_tensor(out=c, in0=c, in1=mu[:], op=ALU.subtract), r=[xk(f)] + stat_keys, w=[xk(f)])
            tk.op("dve", lambda e: e.tensor_tensor(out=c, in0=c, in1=rstd[:], op=ALU.mult), r=[xk(f)] + stat_keys, w=[xk(f)])
            tk.op("act", lambda e: e.activation(out=bufA[:, f, 0:T], in_=c, func=AF.Silu, bias=lb_[:, f:f + 1], scale=lg_[:, f:f + 1]),
                  r=[xk(f), "lng", "lnb"], w=[kA(f)])
        xv = io["xT"].rearrange("(kc p) t -> p kc t", p=128)
        for f in range(KC):
            tk.dma("sp", xs[:, f, halo:halo + T], xv[:, f, halo:halo + T], w=[xk(f)])
        epilogue(cx, xs, halo, bufA, kA, KC, w_out, modA, io["modB"], io["gB"], io["router"], io, bufB, kB)
        tk.finish()
    return nc


def run_m_conv(xT_full, inp, mod, layer):
    nc = get_nc("m_conv", build_m_conv)
    halo = CONV_HALO
    maps = common_maps(xT_full, inp, mod, layer, halo)
    dw = inp["conv_dw"][0]
    dwp = np.ascontiguousarray(dw.reshape(31, KC, 128).transpose(2, 1, 0).reshape(128, KC * 31))
    for r in range(NCORES):
        lo = r * T - halo
        left = ((np.arange(lo, lo + halo) >= 0)).astype(np.float32)
        right = ((np.arange((r + 1) * T, (r + 1) * T + halo) < S)).astype(np.float32)
        vme = np.concatenate([np.tile(left, KC), np.tile(right, KC)])[None, :]
        maps[r].update({
            "w_in": np.ascontiguousarray(inp["conv_w_in"][0]), "dw": dwp,
            "lng": fm16(inp["conv_ln_g"][0]), "lnb": fm16(inp["conv_ln_b"][0]),
            "w_out": np.ascontiguousarray(inp["conv_w_out"][0]), "vme": np.ascontiguousarray(vme),
            "ident": np.eye(128, dtype=np.float32),
        })
    return gather_common(run(nc, maps))


TWO_PI_HI = 6.28125
TWO_PI_LO = 2.0 * np.pi - 6.28125
PI_SAFE = 3.1415925


def rope_tables(cx, pos_ap, inv_ap, npart, n):
    tk = cx.tk
    pi_ = tk.sb([npart, n], I32)
    ang = tk.sb([npart, n], F32)
    ki = tk.sb([npart, n], I32)
    kf = tk.sb([npart, n], F32)
    inv = tk.sb([npart, 1], F32)
    cos = tk.sb([npart, n], F32, name="rope_cos")
    sin = tk.sb([npart, n], F32, name="rope_sin")
    tk.dma("sp", pi_[:], pos_ap.to_broadcast([npart, n]), w=["rp_pi"])
    tk.dma("sp", inv[:], inv_ap, w=["rp_inv"])
    tk.op("dve", lambda e: e.tensor_copy(ang[:], pi_[:]), r=["rp_pi"], w=["rp_ang"])
    tk.op("dve", lambda e: e.tensor_scalar(out=ang[:], in0=ang[:], scalar1=inv[:, 0:1], scalar2=None, op0=ALU.mult),
          r=["rp_ang", "rp_inv"], w=["rp_ang"])
    for (dst, shift, key) in ((sin, 0.0, "rope_sin"), (cos, float(np.pi / 2), "rope_cos")):
        if shift != 0.0:
            tk.op("dve", lambda e: e.tensor_scalar(out=dst[:], in0=ang[:], scalar1=shift, scalar2=None, op0=ALU.add), r=["rp_ang"], w=[key])
            src, sk = dst, key
        else:
            src, sk = ang, "rp_ang"
        tk.op("dve", lambda e, src=src: e.tensor_scalar(out=ki[:], in0=src[:], scalar1=float(1.0 / (2 * np.pi)), scalar2=None, op0=ALU.mult),
              r=[sk], w=["rp_ki"])
        tk.op("dve", lambda e: e.tensor_copy(kf[:], ki[:]), r=["rp_ki"], w=["rp_kf"])
        tk.op("dve", lambda e, src=src, dst=dst: e.scalar_tensor_tensor(out=dst[:], in0=kf[:], scalar=-TWO_PI_HI, in1=src[:], op0=ALU.mult, op1=ALU.add),
              r=["rp_kf", sk], w=[key])
        tk.op("dve", lambda e, dst=dst: e.scalar_tensor_tensor(out=dst[:], in0=kf[:], scalar=-TWO_PI_LO, in1=dst[:], op0=ALU.mult, op1=ALU.add),
              r=["rp_kf", key], w=[key])
        tk.op("dve", lambda e, dst=dst: e.tensor_scalar(out=dst[:], in0=dst[:], scalar1=PI_SAFE, scalar2=-PI_SAFE, op0=ALU.min, op1=ALU.max),
              r=[key], w=[key])
        tk.op("act", lambda e, dst=dst: e.activation(out=dst[:], in_=dst[:], func=AF.Sin), r=[key], w=[key])
    return cos, sin


def rot_matrix(half):
    n = 2 * half
    R = np.zeros((n, n), np.float32)
    for m in range(half):
        R[m + half, m] = -1.0
        R[m, m + half] = 1.0
    return R


def inv_freq(half, npart):
    inv = 10000.0 ** (-np.arange(half, dtype=np.float32) / half)
    return np.ascontiguousarray(np.tile(inv, npart // half)[:, None].astype(np.float32))


def build_ma_dil():
    nc = new_nc()
    xT = din(nc, "xT", [D, T], F32)
    modA_ap = din(nc, "modA", [128, 48], F32)
    gA = din(nc, "gA", [128, 16], F32)
    w_in = din(nc, "w_in", [D, 9216], F32)
    pos = din(nc, "pos", [1, T], I32)
    invf = din(nc, "invf", [128, 1], F32)
    rm_ap = din(nc, "rotm", [128, 128], F32)
    gq_ap = din(nc, "gq", [128, 3], F32)
    gk_ap = din(nc, "gk", [128, 3], F32)
    qkv = dout(nc, "qkv", [72, 128, T], BF16)
    es = ExitStack()
    with es:
        cx = Ctx(nc, es, n_wbuf=3, n_ps=8, n_psb=0)
        tk = cx.tk
        xs = load_xT(cx, xT, T)
        modA = load_mod(cx, modA_ap, gA)
        cos, sin = rope_tables(cx, pos, invf, 128, T)
        rmb = tk.sb([128, 128], BF16)
        tk.dma("pool", rmb[:], rm_ap, w=["rmb"])
        gq = tk.sb([128, 3], F32)
        gk = tk.sb([128, 3], F32)
        tk.dma("sp", gq[:], gq_ap, w=["gq"])
        tk.dma("sp", gk[:], gk_ap, w=["gk"])
        eps1 = tk.sb([128, 1], F32)
        tk.op("dve", lambda e: e.memset(eps1[:], float(EPS)), w=["eps1"])
        hT = tk.sb([128, KC, T], BF16, name="hT")
        kA = lambda kc: "A%d" % kc
        rms_mod(cx, xs, lambda kc: "x%d" % kc, T, 0, modA, hT, kA)
        qg = [tk.sb([128, 512], F32) for _ in range(2)]
        qgb = [tk.sb([128, 512], BF16) for _ in range(2)]
        t1 = [tk.sb([128, 512], F32) for _ in range(2)]
        ob = [tk.sb([128, 512], BF16) for _ in range(3)]
        cnt = [0, 0]

        def evac(ci, c0, M, tc0, n, ps, pkey):
            g = ci // 24
            kind = (ci // 8) % 3
            o_ = ob[cnt[1] % 3]
            ok = "ob%d" % (cnt[1] % 3)
            cnt[1] += 1
            if kind == 2:
                tk.op("act", lambda e: e.activation(out=o_[:, 0:n], in_=ps[:, 0:n], func=AF.Copy), r=[pkey], w=[ok])
            else:
                i = cnt[0] % 2
                cnt[0] += 1
                gain = gq if kind == 0 else gk
                gkey = "gq" if kind == 0 else "gk"
                sq, sqk = cx.rm_sq[i], "rmsq%d" % i
                tk.op("act", lambda e: e.activation(out=sq[:, 0:n], in_=ps[:, 0:n], func=AF.Square), r=[pkey], w=[sqk])
                pn, pnk = cx.next_ps()
                tk.op("pe", lambda e: e.matmul(pn[:, 0:n], cx.ones[:], sq[:, 0:n], start=True, stop=True), r=[sqk, "ones"], w=[pnk])
                rs, rsk = cx.rm_tmp[i], "rmtmp%d" % i
                tk.op("act", lambda e: e.activation(out=rs[:, 0:n], in_=pn[:, 0:n], func=AF.Sqrt, bias=eps1[:, 0:1], scale=1.0 / 128),
                      r=[pnk, "eps1"], w=[rsk])
                tk.op("dve", lambda e: e.reciprocal(out=rs[:, 0:n], in_=rs[:, 0:n]), r=[rsk], w=[rsk])
                q_, qk = qg[i], "qg%d" % i
                tk.op("dve", lambda e: e.scalar_tensor_tensor(out=q_[:, 0:n], in0=ps[:, 0:n], scalar=gain[:, g:g + 1], in1=rs[:, 0:n],
                                                              op0=ALU.mult, op1=ALU.mult), r=[pkey, gkey, rsk], w=[qk])
                qb_, qbk = qgb[i], "qgb%d" % i
                tk.op("act", lambda e: e.activation(out=qb_[:, 0:n], in_=q_[:, 0:n], func=AF.Copy), r=[qk], w=[qbk])
                pr, prk = cx.next_ps()
                tk.op("pe", lambda e: e.matmul(pr[:, 0:n], rmb[:], qb_[:, 0:n], start=True, stop=True), r=[qbk, "rmb"], w=[prk])
                t_, tkk = t1[i], "t1_%d" % i
                tk.op("dve", lambda e: e.tensor_tensor(out=t_[:, 0:n], in0=q_[:, 0:n], in1=cos[:, tc0:tc0 + n], op=ALU.mult),
                      r=[qk, "rope_cos"], w=[tkk])
                tk.op("dve", lambda e: e.tensor_tensor(out=q_[:, 0:n], in0=pr[:, 0:n], in1=sin[:, tc0:tc0 + n], op=ALU.mult),
                      r=[prk, "rope_sin", qk, qbk], w=[qk])
                tk.op("dve", lambda e: e.tensor_tensor(out=o_[:, 0:n], in0=t_[:, 0:n], in1=q_[:, 0:n], op=ALU.add), r=[tkk, qk], w=[ok])
            tk.dma("sp", qkv[ci, :, tc0:tc0 + n], o_[:, 0:n], r=[ok], is_output=True)

        linear_fm(cx, w_in, hT, kA, KC, [(c * 128, 128) for c in range(72)], T, evac)
        tk.finish()
    return nc


DIL = ((128, 1), (512, 4), (2048, 16))


def build_mb_dil():
    nc = new_nc()
    Qs, Ks, Vs, VMs = [], [], [], []
    for g, (w, d) in enumerate(DIL):
        L = S // d
        Qs.append(din(nc, "Q%d" % g, [128, S], BF16))
        Ks.append(din(nc, "K%d" % g, [128, d * (L + 128)], BF16))
        Vs.append(din(nc, "V%d" % g, [d * (L + 128), 128], BF16))
        VMs.append(din(nc, "VM%d" % g, [d * (L + 128), 128], BF16))
    band_ap = din(nc, "band", [128, 256], F32)
    oT = dout(nc, "oT", [128, S], BF16)
    es = ExitStack()
    with es:
        cx = Ctx(nc, es, n_wbuf=0, n_ps=8, n_psb=0)
        tk = cx.tk
        band = tk.sb([128, 256], BF16)
        tk.dma("pool", band[:], band_ap, w=["band"])
        numer = tk.sb([128, S], F32, name="numer")
        den = tk.sb([128, S], F32, name="den")
        Qsb = tk.sb([128, S], BF16, name="Qsb")
        Ksb = tk.sb([128, 16 * (512 + 128)], BF16, name="Ksb")
        Vsb = tk.sb([128, 80, 128], BF16, name="Vsb")
        VMsb = tk.sb([128, 80, 128], BF16, name="VMsb")
        pe_ = [tk.sb([128, 256], BF16) for _ in range(4)]
        pm_ = [tk.sb([128, 256], BF16) for _ in range(4)]
        scale = float(128 ** -0.5)
        ui = 0
        for g, (w, d) in enumerate(DIL):
            L = S // d
            Lh = L + 128
            ntile = d * Lh // 128
            tk.dma("sp", Qsb[:], Qs[g], w=["Q"])
            tk.dma("sp", Ksb[:, 0:d * Lh], Ks[g], w=["K"])
            Vv = Vs[g].rearrange("(n p) c -> p n c", p=128)
            VMv = VMs[g].rearrange("(n p) c -> p n c", p=128)
            for a in range(0, ntile, 16):
                b = min(ntile, a + 16)
                tk.dma("sp", Vsb[:, a:b, :], Vv[:, a:b, :], w=["V%d" % (a // 16)])
                tk.dma("sp", VMsb[:, a:b, :], VMv[:, a:b, :], w=["VM%d" % (a // 16)])
            vkeys = ["V%d" % i for i in range(5)] + ["VM%d" % i for i in range(5)]
            units = [(p, qt) for p in range(d) for qt in range(L // 128)]
            LA = 2
            pend = {}
            for step in range(len(units) + LA):
                if step < len(units):
                    p, qt = units[step]
                    i3 = ui % 4
                    ui += 1
                    ps_s, ks_ = cx.next_ps()
                    qcol = p * L + qt * 128
                    for j in range(2):
                        kcol = p * Lh + (qt + j) * 128
                        tk.op("pe", lambda e, j=j, kcol=kcol, ps_s=ps_s, qcol=qcol: e.matmul(ps_s[:, j * 128:(j + 1) * 128], Ksb[:, kcol:kcol + 128],
                                                                                            Qsb[:, qcol:qcol + 128], start=True, stop=True),
                              r=["Q", "K"], w=[ks_])
                    pe, pek = pe_[i3], "pe%d" % i3
                    tk.op("act", lambda e, pe=pe, ps_s=ps_s: e.activation(out=pe[:], in_=ps_s[:, 0:256], func=AF.Exp, scale=scale), r=[ks_], w=[pek])
                    pm, pmk = pm_[i3], "pm%d" % i3
                    tk.op("pool", lambda e, pm=pm, pe=pe: e.tensor_tensor(out=pm[:], in0=pe[:], in1=band[:], op=ALU.mult), r=[pek, "band"], w=[pmk])
                    pend[step] = (pm, pmk)
                if step >= LA:
                    p, qt = units[step - LA]
                    pm, pmk = pend.pop(step - LA)
                    ps_o, ko_ = cx.next_ps()
                    ps_d, kd_ = cx.next_ps()
                    for j in range(2):
                        tile = (p * Lh) // 128 + qt + j
                        tk.op("pe", lambda e, j=j, tile=tile, pm=pm, ps_o=ps_o: e.matmul(ps_o[:, 0:128], Vsb[:, tile, :], pm[:, j * 128:(j + 1) * 128],
                                                                                        start=(j == 0), stop=(j == 1)), r=[pmk] + vkeys, w=[ko_])
                    for j in range(2):
                        tile = (p * Lh) // 128 + qt + j
                        tk.op("pe", lambda e, j=j, tile=tile, pm=pm, ps_d=ps_d: e.matmul(ps_d[:, 0:128], VMsb[:, tile, :], pm[:, j * 128:(j + 1) * 128],
                                                                                        start=(j == 0), stop=(j == 1)), r=[pmk] + vkeys, w=[kd_])
                    t0 = qt * 128 * d + p
                    nsl = numer[:, t0:t0 + 127 * d + 1:d] if d > 1 else numer[:, t0:t0 + 128]
                    dsl = den[:, t0:t0 + 127 * d + 1:d] if d > 1 else den[:, t0:t0 + 128]
                    blk = "nd%d" % ((qt * 128 * d) // 2048)
                    if g == 0:
                        tk.op("dve", lambda e, nsl=nsl, ps_o=ps_o: e.tensor_copy(nsl, ps_o[:, 0:128]), r=[ko_], w=[blk + "n"])
                        tk.op("dve", lambda e, dsl=dsl, ps_d=ps_d: e.tensor_copy(dsl, ps_d[:, 0:128]), r=[kd_], w=[blk + "d"])
                    else:
                        tk.op("dve", lambda e, nsl=nsl, ps_o=ps_o: e.tensor_tensor(out=nsl, in0=nsl, in1=ps_o[:, 0:128], op=ALU.add), r=[ko_, blk + "n"], w=[blk + "n"])
                        tk.op("dve", lambda e, dsl=dsl, ps_d=ps_d: e.tensor_tensor(out=dsl, in0=dsl, in1=ps_d[:, 0:128], op=ALU.add), r=[kd_, blk + "d"], w=[blk + "d"])
        ob = [tk.sb([128, 512], BF16) for _ in range(2)]
        for c in range(S // 512):
            blk = "nd%d" % ((c * 512) // 2048)
            o_, ok = ob[c % 2], "ob%d" % (c % 2)
            tk.op("dve", lambda e: e.reciprocal(out=den[:, c * 512:(c + 1) * 512], in_=den[:, c * 512:(c + 1) * 512]), r=[blk + "d"], w=[blk + "d"])
            tk.op("dve", lambda e: e.tensor_tensor(out=o_[:], in0=numer[:, c * 512:(c + 1) * 512], in1=den[:, c * 512:(c + 1) * 512], op=ALU.mult),
                  r=[blk + "n", blk + "d"], w=[ok])
            tk.dma("sp", oT[:, c * 512:(c + 1) * 512], o_[:], r=[ok], is_output=True)
        tk.finish()
    return nc


def build_mc(nk):
    nc = new_nc()
    io = m_common_io(nc, 0)
    oT = din(nc, "oT", [nk * 128, T], BF16)
    w_out = din(nc, "w_out", [nk * 128, D], F32)
    es = ExitStack()
    with es:
        cx = Ctx(nc, es, n_wbuf=3)
        tk = cx.tk
        xs = load_xT(cx, io["xT"], T)
        modA = load_mod(cx, io["modA"], io["gA"])
        bufA = tk.sb([128, KC, T], BF16, name="bufA")
        bufB = tk.sb([128, KC, T], BF16, name="bufB")
        kA = lambda kc: "A%d" % kc
        kB = lambda kc: "B%d" % kc
        ov = oT.rearrange("(kc p) t -> p kc t", p=128)
        for kc in range(nk):
            tk.dma("sp", bufA[:, kc, :], ov[:, kc, :], w=[kA(kc)])
        epilogue(cx, xs, 0, bufA, kA, nk, w_out, modA, io["modB"], io["gB"], io["router"], io, bufB, kB)
        tk.finish()
    return nc


def run_dil_layer(xT_full, inp, mod, layer, positions):
    nc = get_nc("ma_dil", build_ma_dil)
    maps = []
    for r in range(NCORES):
        maps.append({
            "xT": np.ascontiguousarray(xT_full[:, r * T:(r + 1) * T]),
            "modA": mod48(mod[layer, 0]), "gA": fm16(inp["norm_g"][layer, 0]),
            "w_in": np.ascontiguousarray(inp["dil_w_in"][0]),
            "pos": np.ascontiguousarray(positions[:, r * T:(r + 1) * T]).astype(np.int32),
            "invf": inv_freq(64, 128), "rotm": rot_matrix(64),
            "gq": np.ascontiguousarray(inp["dil_q_norm"][0].T), "gk": np.ascontiguousarray(inp["dil_k_norm"][0].T),
        })
    res = run(nc, maps)
    qkv = np.concatenate([res[r]["qkv"] for r in range(NCORES)], axis=2)
    nc = get_nc("mb_dil", build_mb_dil)
    kk = np.arange(128)[:, None]
    ii = np.arange(128)[None, :]
    band = np.concatenate([(np.abs(ii + 64 - 128 * j - kk) <= 64).astype(np.float32) for j in range(2)], axis=1)
    maps = []
    for h in range(NCORES):
        m = {"band": band}
        for g, (w, d) in enumerate(DIL):
            L = S // d
            q = qkv[(g * 3 + 0) * 8 + h]
            k = qkv[(g * 3 + 1) * 8 + h]
            v = qkv[(g * 3 + 2) * 8 + h]
            m["Q%d" % g] = np.ascontiguousarray(q.reshape(128, L, d).transpose(0, 2, 1).reshape(128, S))
            kp = np.zeros((128, d, L + 128), q.dtype)
            kp[:, :, 64:64 + L] = k.reshape(128, L, d).transpose(0, 2, 1)
            m["K%d" % g] = kp.reshape(128, d * (L + 128))
            vp = np.zeros((d, L + 128, 128), q.dtype)
            vp[:, 64:64 + L, :] = v.T.reshape(L, d, 128).transpose(1, 0, 2)
            m["V%d" % g] = vp.reshape(d * (L + 128), 128)
            vm = np.zeros((d, L + 128, 128), q.dtype)
            vm[:, 64:64 + L, :] = 1
            m["VM%d" % g] = vm.reshape(d * (L + 128), 128)
        maps.append(m)
    res = run(nc, maps)
    oT = np.concatenate([res[h]["oT"] for h in range(NCORES)], axis=0)
    return run_mc(xT_full, oT, inp["dil_w_out"][0], inp, mod, layer, 8)


def run_mc(xT_full, oT, w_out, inp, mod, layer, nk):
    nc = get_nc("mc%d" % nk, lambda: build_mc(nk))
    maps = common_maps(xT_full, inp, mod, layer, 0)
    for r in range(NCORES):
        maps[r].update({"oT": np.ascontiguousarray(oT[:, r * T:(r + 1) * T]), "w_out": np.ascontiguousarray(w_out)})
    return gather_common(run(nc, maps))


def build_ma_mla():
    nc = new_nc()
    xT = din(nc, "xT", [D, T], F32)
    modA_ap = din(nc, "modA", [128, 48], F32)
    gA = din(nc, "gA", [128, 16], F32)
    w_in = din(nc, "w_in", [D, 1088], F32)
    w_q_up = din(nc, "w_q_up", [512, 3072], F32)
    w_kv_up = din(nc, "w_kv_up", [512, 4096], F32)
    pos = din(nc, "pos", [1, T], I32)
    invf = din(nc, "invf", [64, 1], F32)
    rm_ap = din(nc, "rotm", [64, 64], F32)
    ga_ap = din(nc, "ga", [128, 8], F32)
    gqk_ap = din(nc, "gqk", [128, 4], F32)
    QnT = dout(nc, "QnT", [16, 128, T], BF16)
    QrT = dout(nc, "QrT", [16, 64, T], BF16)
    KnT = dout(nc, "KnT", [16, 128, T], BF16)
    KrT = dout(nc, "KrT", [16, 64, T], BF16)
    VT = dout(nc, "VT", [16, 128, T], BF16)
    es = ExitStack()
    with es:
        cx = Ctx(nc, es, n_wbuf=2, n_ps=8, n_psb=0)
        tk = cx.tk
        xs = load_xT(cx, xT, T)
        modA = load_mod(cx, modA_ap, gA)
        cos, sin = rope_tables(cx, pos, invf, 64, T)
        rmb = tk.sb([64, 64], BF16)
        tk.dma("pool", rmb[:], rm_ap, w=["rmb"])
        ga = tk.sb([128, 8], F32)
        gqk = tk.sb([128, 4], F32)
        tk.dma("sp", ga[:], ga_ap, w=["ga"])
        tk.dma("sp", gqk[:], gqk_ap, w=["gqk"])
        eps1 = tk.sb([128, 1], F32)
        tk.op("dve", lambda e: e.memset(eps1[:], float(EPS)), w=["eps1"])
        bufA = tk.sb([128, KC, T], BF16, name="bufA")
        kA = lambda kc: "A%d" % kc
        rms_mod(cx, xs, lambda kc: "x%d" % kc, T, 0, modA, bufA, kA)
        cf = xs
        ck = lambda j: "x%d" % j

        def evac_lat(ci, c0, M, tc0, n, ps, pkey):
            tk.op("act" if ci % 2 == 0 else "dve",
                  (lambda e: e.activation(out=cf[0:M, ci, tc0:tc0 + n], in_=ps[0:M, 0:n], func=AF.Copy)) if ci % 2 == 0 else
                  (lambda e: e.tensor_copy(cf[0:M, ci, tc0:tc0 + n], ps[0:M, 0:n])), r=[pkey], w=[ck(ci)])
        linear_fm(cx, w_in, bufA, kA, KC, [(j * 128, 128) for j in range(8)] + [(1024, 64)], T, evac_lat)
        bufB = tk.sb([128, 8, T], BF16, name="bufB")
        kB = lambda j: "B%d" % j
        for grp in range(2):
            for ti, (tc0, n) in enumerate(tchunks(T)):
                pn, pnk = cx.next_ps()
                for j in range(4):
                    sq, sqk = cx.rm_sq[j % 2], "rmsq%d" % (j % 2)
                    tk.op("act", lambda e: e.activation(out=sq[:, 0:n], in_=cf[:, grp * 4 + j, tc0:tc0 + n], func=AF.Square),
                          r=[ck(grp * 4 + j)], w=[sqk])
                    tk.op("pe", lambda e: e.matmul(pn[:, 0:n], cx.ones[:], sq[:, 0:n], start=(j == 0), stop=(j == 3)), r=[sqk, "ones"], w=[pnk])
                rs, rsk = cx.rm_rstd, "rmrstd"
                tk.op("act", lambda e: e.activation(out=rs[:, 0:n], in_=pn[:, 0:n], func=AF.Sqrt, bias=eps1[:, 0:1], scale=1.0 / 512),
                      r=[pnk, "eps1"], w=[rsk])
                tk.op("dve", lambda e: e.reciprocal(out=rs[:, 0:n], in_=rs[:, 0:n]), r=[rsk], w=[rsk])
                for j in range(4):
                    jj = grp * 4 + j
                    tk.op("dve", lambda e, jj=jj: e.scalar_tensor_tensor(out=bufB[:, jj, tc0:tc0 + n], in0=cf[:, jj, tc0:tc0 + n],
                                                                         scalar=ga[:, jj:jj + 1], in1=rs[:, 0:n], op0=ALU.mult, op1=ALU.mult),
                          r=[ck(jj), "ga", rsk], w=[kB(jj)])
        for ti, (tc0, n) in enumerate(tchunks(T)):
            sq, sqk = cx.rm_sq[ti % 2], "rmsq%d" % (ti % 2)
            tk.op("act", lambda e: e.activation(out=sq[0:64, 0:n], in_=cf[0:64, 8, tc0:tc0 + n], func=AF.Square), r=[ck(8)], w=[sqk])
            pn, pnk = cx.next_ps()
            tk.op("pe", lambda e: e.matmul(pn[:, 0:n], cx.ones[0:64, :], sq[0:64, 0:n], start=True, stop=True), r=[sqk, "ones"], w=[pnk])
            tk.op("dve", lambda e: e.tensor_copy(cf[:, 9, tc0:tc0 + n], pn[:, 0:n]), r=[pnk], w=[ck(9)])

        qg = [tk.sb([64, 512], F32) for _ in range(2)]
        qgb = [tk.sb([64, 512], BF16) for _ in range(2)]
        t1 = [tk.sb([64, 512], F32) for _ in range(2)]
        ob = [tk.sb([128, 512], BF16) for _ in range(4)]
        cnt = [0, 0]

        def nxt_ob():
            o_ = ob[cnt[1] % 4]
            ok = "ob%d" % (cnt[1] % 4)
            cnt[1] += 1
            return o_, ok

        def rope64(src_ap, src_keys, gcol, rs, rsk, tc0, n, dst_dram):
            i = cnt[0] % 2
            cnt[0] += 1
            q_, qk = qg[i], "qg%d" % i
            tk.op("dve", lambda e: e.scalar_tensor_tensor(out=q_[:, 0:n], in0=src_ap, scalar=gqk[0:64, gcol:gcol + 1], in1=rs[0:64, 0:n],
                                                          op0=ALU.mult, op1=ALU.mult), r=src_keys + ["gqk", rsk], w=[qk])
            qb_, qbk = qgb[i], "qgb%d" % i
            tk.op("act", lambda e: e.activation(out=qb_[:, 0:n], in_=q_[:, 0:n], func=AF.Copy), r=[qk], w=[qbk])
            pr, prk = cx.next_ps()
            tk.op("pe", lambda e: e.matmul(pr[0:64, 0:n], rmb[:], qb_[:, 0:n], start=True, stop=True), r=[qbk, "rmb"], w=[prk])
            t_, tkk = t1[i], "t1_%d" % i
            tk.op("dve", lambda e: e.tensor_tensor(out=t_[:, 0:n], in0=q_[:, 0:n], in1=cos[:, tc0:tc0 + n], op=ALU.mult), r=[qk, "rope_cos"], w=[tkk])
            tk.op("dve", lambda e: e.tensor_tensor(out=q_[:, 0:n], in0=pr[0:64, 0:n], in1=sin[:, tc0:tc0 + n], op=ALU.mult),
                  r=[prk, "rope_sin", qk, qbk], w=[qk])
            o_, ok = nxt_ob()
            tk.op("dve", lambda e: e.tensor_tensor(out=o_[0:64, 0:n], in0=t_[:, 0:n], in1=q_[:, 0:n], op=ALU.add), r=[tkk, qk], w=[ok])
            tk.dma("sp", dst_dram, o_[0:64, 0:n], r=[ok], is_output=True)

        held = {}

        def evac_q(ci, c0, M, tc0, n, ps, pkey):
            hd, part = divmod(ci, 2)
            if part == 0:
                held[(hd, tc0)] = (ps, pkey)
                return
            pn_, pnk_ = held.pop((hd, tc0))
            sq, sqk = cx.rm_sq[0], "rmsq0"
            sq2, sq2k = cx.rm_sq[1], "rmsq1"
            tk.op("act", lambda e: e.activation(out=sq[:, 0:n], in_=pn_[:, 0:n], func=AF.Square), r=[pnk_], w=[sqk])
            tk.op("act", lambda e: e.activation(out=sq2[0:64, 0:n], in_=ps[0:64, 0:n], func=AF.Square), r=[pkey], w=[sq2k])
            pss, pssk = cx.next_ps()
            tk.op("pe", lambda e: e.matmul(pss[:, 0:n], cx.ones[:], sq[:, 0:n], start=True, stop=False), r=[sqk, "ones"], w=[pssk])
            tk.op("pe", lambda e: e.matmul(pss[:, 0:n], cx.ones[0:64, :], sq2[0:64, 0:n], start=False, stop=True), r=[sq2k, "ones"], w=[pssk])
            rs, rsk = cx.rm_rstd, "rmrstd"
            tk.op("act", lambda e: e.activation(out=rs[:, 0:n], in_=pss[:, 0:n], func=AF.Sqrt, bias=eps1[:, 0:1], scale=1.0 / 192),
                  r=[pssk, "eps1"], w=[rsk])
            tk.op("dve", lambda e: e.reciprocal(out=rs[:, 0:n], in_=rs[:, 0:n]), r=[rsk], w=[rsk])
            o_, ok = nxt_ob()
            tk.op("dve", lambda e: e.scalar_tensor_tensor(out=o_[:, 0:n], in0=pn_[:, 0:n], scalar=gqk[:, 0:1], in1=rs[:, 0:n],
                                                          op0=ALU.mult, op1=ALU.mult), r=[pnk_, "gqk", rsk], w=[ok])
            tk.dma("sp", QnT[hd, :, tc0:tc0 + n], o_[:, 0:n], r=[ok], is_output=True)
            rope64(ps[0:64, 0:n], [pkey], 1, rs, rsk, tc0, n, QrT[hd, :, tc0:tc0 + n])

        chunks = []
        for hd in range(16):
            chunks += [(192 * hd, 128), (192 * hd + 128, 64)]
        linear_fm(cx, w_q_up, bufB[:, 0:4, :], lambda kc: kB(kc), 4, chunks, T, evac_q)

        def evac_kv(ci, c0, M, tc0, n, ps, pkey):
            hd, part = divmod(ci, 2)
            if part == 1:
                o_, ok = nxt_ob()
                tk.op("act", lambda e: e.activation(out=o_[:, 0:n], in_=ps[:, 0:n], func=AF.Copy), r=[pkey], w=[ok])
                tk.dma("sp", VT[hd, :, tc0:tc0 + n], o_[:, 0:n], r=[ok], is_output=True)
                return
            sq, sqk = cx.rm_sq[0], "rmsq0"
            tk.op("act", lambda e: e.activation(out=sq[:, 0:n], in_=ps[:, 0:n], func=AF.Square), r=[pkey], w=[sqk])
            pss, pssk = cx.next_ps()
            tk.op("pe", lambda e: e.matmul(pss[:, 0:n], cx.ones[:], sq[:, 0:n], start=True, stop=True), r=[sqk, "ones"], w=[pssk])
            rs, rsk = cx.rm_rstd, "rmrstd"
            tk.op("dve", lambda e: e.tensor_tensor(out=rs[:, 0:n], in0=pss[:, 0:n], in1=cf[:, 9, tc0:tc0 + n], op=ALU.add),
                  r=[pssk, ck(9)], w=[rsk])
            tk.op("act", lambda e: e.activation(out=rs[:, 0:n], in_=rs[:, 0:n], func=AF.Sqrt, bias=eps1[:, 0:1], scale=1.0 / 192),
                  r=[rsk, "eps1"], w=[rsk])
            tk.op("dve", lambda e: e.reciprocal(out=rs[:, 0:n], in_=rs[:, 0:n]), r=[rsk], w=[rsk])
            o_, ok = nxt_ob()
            tk.op("dve", lambda e: e.scalar_tensor_tensor(out=o_[:, 0:n], in0=ps[:, 0:n], scalar=gqk[:, 2:3], in1=rs[:, 0:n],
                                                          op0=ALU.mult, op1=ALU.mult), r=[pkey, "gqk", rsk], w=[ok])
            tk.dma("sp", KnT[hd, :, tc0:tc0 + n], o_[:, 0:n], r=[ok], is_output=True)
            rope64(cf[0:64, 8, tc0:tc0 + n], [ck(8)], 3, rs, rsk, tc0, n, KrT[hd, :, tc0:tc0 + n])

        chunks = []
        for hd in range(16):
            chunks += [(256 * hd, 128), (256 * hd + 128, 128)]
        linear_fm(cx, w_kv_up, bufB[:, 4:8, :], lambda kc: kB(4 + kc), 4, chunks, T, evac_kv)
        tk.finish()
    return nc


def build_mb_mla():
    nc = new_nc()
    Qn = din(nc, "Qn", [2, 128, S], BF16)
    Qr = din(nc, "Qr", [2, 64, S], BF16)
    Kn = din(nc, "Kn", [2, 128, S], BF16)
    Kr = din(nc, "Kr", [2, 64, S], BF16)
    V = din(nc, "V", [2, S, 128], BF16)
    o = dout(nc, "o", [2, S, 128], BF16)
    es = ExitStack()
    with es:
        cx = Ctx(nc, es, n_wbuf=0, n_ps=8, n_psb=0)
        tk = cx.tk
        scale = float(192 ** -0.5)
        sb_ = {}
        for h in range(2):
            sb_[h] = (tk.sb([128, S], BF16), tk.sb([64, S], BF16), tk.sb([128, S], BF16), tk.sb([64, S], BF16), tk.sb([128, 64, 129], BF16))
            qn, qr, kn, kr, vs = sb_[h]
            tk.op("dve", lambda e, vs=vs: e.memset(vs[:, :, 128:129], 1.0), w=["vone%d" % h])
            for c in range(4):
                sl = slice(c * 2048, (c + 1) * 2048)
                tk.dma("sp", qn[:, sl], Qn[h, :, sl], w=["qn%d_%d" % (h, c)])
                tk.dma("sp", qr[:, sl], Qr[h, :, sl], w=["qr%d_%d" % (h, c)])
                tk.dma("sp", kn[:, sl], Kn[h, :, sl], w=["kn%d_%d" % (h, c)])
                tk.dma("sp", kr[:, sl], Kr[h, :, sl], w=["kr%d_%d" % (h, c)])
                tk.dma("sp", vs[:, c * 16:(c + 1) * 16, 0:128], V[h, sl, :].rearrange("(n p) c -> p n c", p=128), w=["v%d_%d" % (h, c)])
        pt = [tk.sb([128, 512], BF16) for _ in range(4)]
        ob = [tk.sb([128, 128], BF16) for _ in range(4)]
        rd = [tk.sb([128, 1], F32) for _ in range(4)]
        pi = 0
        qi = 0
        oi = 0
        for h in range(2):
            qn, qr, kn, kr, vs = sb_[h]
            for qc in range(S // 512):
                qs = slice(qc * 512, (qc + 1) * 512)
                c4 = qc // 4
                banks = [(cx.psum[4 + b], "psum%d" % (4 + b)) for b in range(4)]
                qi += 1
                LA = 2
                pend = {}
                for step in range(64 + LA):
                    if step < 64:
                        kt = step
                        ks = slice(kt * 128, (kt + 1) * 128)
                        kc4 = kt // 16
                        ps_, psk = cx.psum[pi % 4], "psum%d" % (pi % 4)
                        p_, pk = pt[pi % 4], "pt%d" % (pi % 4)
                        pi += 1
                        tk.op("pe", lambda e, ps_=ps_, ks=ks: e.matmul(ps_[:, :], kn[:, ks], qn[:, qs], start=True, stop=False),
                              r=["kn%d_%d" % (h, kc4), "qn%d_%d" % (h, c4)], w=[psk])
                        tk.op("pe", lambda e, ps_=ps_, ks=ks: e.matmul(ps_[:, :], kr[:, ks], qr[:, qs], start=False, stop=True),
                              r=["kr%d_%d" % (h, kc4), "qr%d_%d" % (h, c4)], w=[psk])
                        tk.op("act", lambda e, ps_=ps_, p_=p_: e.activation(out=p_[:], in_=ps_[:], func=AF.Exp, scale=scale), r=[psk], w=[pk])
                        pend[kt] = (p_, pk, kc4)
                    if step >= LA:
                        kt = step - LA
                        p_, pk, kc4 = pend.pop(kt)
                        for sbk in range(4):
                            bk, bkk = banks[sbk]
                            c0 = 0
                            tk.op("pe", lambda e, p_=p_, kt=kt, bk=bk, c0=c0, sbk=sbk: e.matmul(bk[:, c0:c0 + 129], p_[:, sbk * 128:(sbk + 1) * 128],
                                                                                                vs[:, kt, :], start=(kt == 0), stop=(kt == 63)),
                                  r=[pk, "v%d_%d" % (h, kc4), "vone%d" % h], w=[bkk])
                for sbk in range(4):
                    bk, bkk = banks[sbk]
                    c0 = 0
                    r_, rk = rd[oi % 4], "rd%d" % (oi % 4)
                    o_, ok = ob[oi % 4], "ob%d" % (oi % 4)
                    oi += 1
                    tk.op("dve", lambda e, bk=bk, c0=c0, r_=r_: e.reciprocal(out=r_[:], in_=bk[:, c0 + 128:c0 + 129]), r=[bkk], w=[rk])
                    tk.op("dve", lambda e, bk=bk, c0=c0, r_=r_, o_=o_: e.tensor_scalar(out=o_[:], in0=bk[:, c0:c0 + 128], scalar1=r_[:, 0:1], scalar2=None,
                                                                                      op0=ALU.mult), r=[bkk, rk], w=[ok])
                    q0 = qc * 512 + sbk * 128
                    tk.dma("sp", o[h, q0:q0 + 128, :], o_[:], r=[ok], is_output=True)
        tk.finish()
    return nc


def run_mla_layer(xT_full, inp, mod, layer, positions):
    nc = get_nc("ma_mla", build_ma_mla)
    qn_ = inp["mla_q_norm"][0]
    kn_ = inp["mla_k_norm"][0]
    gqk = np.zeros((128, 4), np.float32)
    gqk[:, 0] = qn_[0:128]
    gqk[0:64, 1] = qn_[128:192]
    gqk[:, 2] = kn_[0:128]
    gqk[0:64, 3] = kn_[128:192]
    ga = np.concatenate([np.asarray(inp["mla_q_a_norm"][0]).reshape(4, 128).T, np.asarray(inp["mla_kv_a_norm"][0]).reshape(4, 128).T], axis=1)
    maps = []
    for r in range(NCORES):
        maps.append({
            "xT": np.ascontiguousarray(xT_full[:, r * T:(r + 1) * T]),
            "modA": mod48(mod[layer, 0]), "gA": fm16(inp["norm_g"][layer, 0]),
            "w_in": np.ascontiguousarray(inp["mla_w_in"][0]), "w_q_up": np.ascontiguousarray(inp["mla_w_q_up"][0]),
            "w_kv_up": np.ascontiguousarray(inp["mla_w_kv_up"][0]),
            "pos": np.ascontiguousarray(positions[:, r * T:(r + 1) * T]).astype(np.int32),
            "invf": inv_freq(32, 64), "rotm": rot_matrix(32),
            "ga": np.ascontiguousarray(ga.astype(np.float32)), "gqk": gqk,
        })
    res = run(nc, maps)
    cat = lambda k: np.concatenate([res[r][k] for r in range(NCORES)], axis=2)
    QnT, QrT, KnT, KrT, VT = cat("QnT"), cat("QrT"), cat("KnT"), cat("KrT"), cat("VT")
    nc = get_nc("mb_mla", build_mb_mla)
    maps = []
    for r in range(NCORES):
        hs = slice(2 * r, 2 * r + 2)
        maps.append({"Qn": np.ascontiguousarray(QnT[hs]), "Qr": np.ascontiguousarray(QrT[hs]),
                     "Kn": np.ascontiguousarray(KnT[hs]), "Kr": np.ascontiguousarray(KrT[hs]),
                     "V": np.ascontiguousarray(VT[hs].transpose(0, 2, 1))})
    res = run(nc, maps)
    oT = np.concatenate([res[r]["o"].transpose(0, 2, 1).reshape(256, S) for r in range(NCORES)], axis=0)
    return run_mc(xT_full, oT, inp["mla_w_out"][0], inp, mod, layer, 16)


def kernel(**inp):
    inp = {k: np.asarray(v) for k, v in inp.items()}
    x = inp["x"][0]
    positions = inp["positions"].astype(np.int32)
    mod = run_ada(inp["c"], inp["ada_w"], inp["ada_b"])
    xT = np.ascontiguousarray(x.T)
    for layer in range(4):
        mixer = layer % 4
        if mixer == 0:
            x1T, h2T, lgT = run_m_conv(xT, inp, mod, layer)
        elif mixer == 1:
            x1T, h2T, lgT = run_m_pool(xT, inp, mod, layer)
        elif mixer == 2:
            x1T, h2T, lgT = run_dil_layer(xT, inp, mod, layer, positions)
        else:
            x1T, h2T, lgT = run_mla_layer(xT, inp, mod, layer, positions)
        Y, inv = run_moe(h2T, lgT, inp, layer)
        xT = run_comb(x1T, Y, inv, np.ascontiguousarray(mod[layer, 1][2 * D:]))
    return np.ascontiguousarray(xT.T)[None].astype(np.float32)
```
